# Optimizing a Trainium2 kernel written in Bass

```python
import numpy as np
import jax
import jax.numpy as jnp
from jax import lax


D_MODEL = 1024
BATCH = 4
SEQ = 4096
DEPTH = 2

PLE_DIM = 256
N_HEADS = 16
HEAD_DIM = 64
ATT_DIM = N_HEADS * HEAD_DIM
NSA_KV_HEADS = 4
NSA_KV_DIM = NSA_KV_HEADS * HEAD_DIM
CMP_BLOCK = 32
CMP_STRIDE = 16
CMP_HIDDEN = 256
SEL_BLOCK = 64
SEL_TOPN = 16
WINDOW = 512
NSA_Q_BLOCK = 64
NSA_PROJ = ATT_DIM + 6 * NSA_KV_DIM + 3 * N_HEADS
MOBA_KV_HEADS = 4
MOBA_KV_DIM = MOBA_KV_HEADS * HEAD_DIM
MOBA_BLOCK = 256
MOBA_TOPK = 3
MOBA_Q_BLOCK = 16
N_GROUPS = 4
EXPERTS_PER_GROUP = 8
N_EXPERTS = N_GROUPS * EXPERTS_PER_GROUP
TOPK_IN_GROUP = 2
D_EXPERT = 128
NORM_EPS = 1e-6
FORCED_SCORE = 1e9

kernel_name = 'hybrid_nsa_moba_yoco_hmoe'


def rms_norm(x, g):
    xf = x.astype(jnp.float32)
    y = xf * lax.rsqrt(jnp.mean(xf * xf, axis=-1, keepdims=True) + NORM_EPS)
    return (y * g.astype(jnp.float32)).astype(x.dtype)


def to_heads(t, n_heads):
    b, s = t.shape[0], t.shape[1]
    return t.reshape(b, s, n_heads, HEAD_DIM).transpose(0, 2, 1, 3)


def alibi_slopes(n_heads):
    return jnp.asarray(2.0 ** (-8.0 * np.arange(1, n_heads + 1) / n_heads), dtype=jnp.float32)


def masked_softmax(s, mask, axis=-1):
    s = jnp.where(mask, s, -jnp.inf)
    m = jnp.max(s, axis=axis, keepdims=True)
    m = jnp.where(jnp.isfinite(m), m, 0.0)
    e = jnp.exp(s - m)
    return e / jnp.maximum(jnp.sum(e, axis=axis, keepdims=True), 1e-30)


def compress_blocks(k, pos, w1, w2):
    b, g, s, d = k.shape
    n_cmp = (s - CMP_BLOCK) // CMP_STRIDE + 1
    idx = np.arange(n_cmp)[:, None] * CMP_STRIDE + np.arange(CMP_BLOCK)[None, :]
    blk = (k[:, :, idx] + pos).reshape(b, g, n_cmp, CMP_BLOCK * d)
    return jax.nn.gelu(blk @ w1) @ w2


def cmp_sel_overlap(n_cmp, n_sel):
    c0 = np.arange(n_cmp)[:, None] * CMP_STRIDE
    s0 = np.arange(n_sel)[None, :] * SEL_BLOCK
    ov = np.clip(np.minimum(c0 + CMP_BLOCK, s0 + SEL_BLOCK) - np.maximum(c0, s0), 0, None)
    return jnp.asarray(ov / CMP_BLOCK, dtype=jnp.float32)


def nsa_attention(xn, w_in, q_gain, k_gain, ck_pos, ck_w1, ck_w2, cv_pos, cv_w1, cv_w2, w_out):
    b, s, _ = xn.shape
    g, r = NSA_KV_HEADS, N_HEADS // NSA_KV_HEADS
    splits = np.cumsum([ATT_DIM] + [NSA_KV_DIM] * 6).tolist()
    q, kc, vc, ks, vs, kw, vw, gl = jnp.split(xn @ w_in, splits, axis=-1)
    q = (rms_norm(to_heads(q, N_HEADS), q_gain) * HEAD_DIM ** -0.5).reshape(b, g, r, s, HEAD_DIM)
    kc = rms_norm(compress_blocks(to_heads(kc, g), ck_pos, ck_w1, ck_w2), k_gain[0])
    vc = compress_blocks(to_heads(vc, g), cv_pos, cv_w1, cv_w2)
    n_cmp = kc.shape[2]
    n_sel = s // SEL_BLOCK
    n_top = min(SEL_TOPN, n_sel)
    ks = rms_norm(to_heads(ks, g), k_gain[1]).reshape(b, g, n_sel, SEL_BLOCK, HEAD_DIM)
    vs = to_heads(vs, g).reshape(b, g, n_sel, SEL_BLOCK, HEAD_DIM)
    pad = ((0, 0), (0, 0), (WINDOW, 0), (0, 0))
    kw = jnp.pad(rms_norm(to_heads(kw, g), k_gain[2]), pad)
    vw = jnp.pad(to_heads(vw, g), pad)
    gates = jax.nn.sigmoid(gl).reshape(b, s, N_HEADS, 3).transpose(0, 2, 1, 3).reshape(b, g, r, s, 3)
    slopes = alibi_slopes(N_HEADS).reshape(1, g, r, 1, 1)
    cmp_end = jnp.arange(n_cmp) * CMP_STRIDE + (CMP_BLOCK - 1)
    overlap = cmp_sel_overlap(n_cmp, n_sel)
    blk_ids = jnp.arange(n_sel)
    bi = jnp.arange(b)[:, None, None, None]
    gi = jnp.arange(g)[None, :, None, None]

    def query_block(c):
        start = c * NSA_Q_BLOCK
        t = start + jnp.arange(NSA_Q_BLOCK)
        qc = lax.dynamic_slice_in_dim(q, start, NSA_Q_BLOCK, axis=3)
        gc = lax.dynamic_slice_in_dim(gates, start, NSA_Q_BLOCK, axis=3)
        dist_c = (t[:, None] - cmp_end[None, :]).astype(jnp.float32)
        s_c = jnp.einsum('bgrqd,bgnd->bgrqn', qc, kc).astype(jnp.float32) - slopes * dist_c
        p_c = masked_softmax(s_c, dist_c >= 0)
        o_c = jnp.einsum('bgrqn,bgnd->bgrqd', p_c.astype(vc.dtype), vc)
        imp = jnp.einsum('bgrqn,nj->bgqj', p_c, overlap)
        cur = (t // SEL_BLOCK)[:, None]
        forced = (blk_ids == 0) | (blk_ids == cur) | (blk_ids == cur - 1)
        imp = jnp.where(blk_ids <= cur, jnp.where(forced, FORCED_SCORE, imp), -jnp.inf)
        _, sel = lax.top_k(imp, n_top)
        k_g = ks[bi, gi, sel]
        v_g = vs[bi, gi, sel]
        pos = sel[..., None] * SEL_BLOCK + jnp.arange(SEL_BLOCK)
        dist_s = (t[:, None, None] - pos)[:, :, None].astype(jnp.float32)
        s_s = jnp.einsum('bgrqd,bgqnld->bgrqnl', qc, k_g).astype(jnp.float32) - slopes[..., None] * dist_s
        p_s = masked_softmax(s_s, dist_s >= 0, axis=(-2, -1))
        o_s = jnp.einsum('bgrqnl,bgqnld->bgrqd', p_s.astype(v_g.dtype), v_g)
        kwc = lax.dynamic_slice_in_dim(kw, start, NSA_Q_BLOCK + WINDOW, axis=2)
        vwc = lax.dynamic_slice_in_dim(vw, start, NSA_Q_BLOCK + WINDOW, axis=2)
        kp = start - WINDOW + jnp.arange(NSA_Q_BLOCK + WINDOW)
        dist_w = t[:, None] - kp[None, :]
        s_w = jnp.einsum('bgrqd,bgkd->bgrqk', qc, kwc).astype(jnp.float32) - slopes * dist_w.astype(jnp.float32)
        p_w = masked_softmax(s_w, (dist_w >= 0) & (dist_w < WINDOW) & (kp[None, :] >= 0))
        o_w = jnp.einsum('bgrqk,bgkd->bgrqd', p_w.astype(vwc.dtype), vwc)
        o = gc[..., 0:1] * o_c + gc[..., 1:2] * o_s + gc[..., 2:3] * o_w
        return o.transpose(0, 3, 1, 2, 4).reshape(b, NSA_Q_BLOCK, ATT_DIM)

    o = lax.map(query_block, jnp.arange(s // NSA_Q_BLOCK))
    o = o.transpose(1, 0, 2, 3).reshape(b, s, ATT_DIM)
    return o @ w_out


def shared_kv(h, kv_norm, w_kv, k_gain):
    b, s, _ = h.shape
    k, v = jnp.split(rms_norm(h, kv_norm) @ w_kv, 2, axis=-1)
    k = rms_norm(to_heads(k, MOBA_KV_HEADS), k_gain)
    v = to_heads(v, MOBA_KV_HEADS)
    nb = -(-s // MOBA_BLOCK)
    pad = ((0, 0), (0, 0), (0, nb * MOBA_BLOCK - s), (0, 0))
    kb = jnp.pad(k, pad).reshape(b, MOBA_KV_HEADS, nb, MOBA_BLOCK, HEAD_DIM)
    vb = jnp.pad(v, pad).reshape(b, MOBA_KV_HEADS, nb, MOBA_BLOCK, HEAD_DIM)
    km = jnp.mean(kb.astype(jnp.float32), axis=3).astype(kb.dtype)
    return kb, vb, km


def moba_attention(xn, kb, vb, km, w_q, q_gain, w_out):
    b, s, _ = xn.shape
    g, r = MOBA_KV_HEADS, N_HEADS // MOBA_KV_HEADS
    q = (rms_norm(to_heads(xn @ w_q, N_HEADS), q_gain) * HEAD_DIM ** -0.5).reshape(b, g, r, s, HEAD_DIM)
    nb = kb.shape[2]
    ktop = min(MOBA_TOPK, nb)
    slopes = alibi_slopes(N_HEADS).reshape(1, g, r, 1, 1)
    bi = jnp.arange(b)[:, None, None, None, None]
    gi = jnp.arange(g)[None, :, None, None, None]
    blk_ids = jnp.arange(nb)
    offs = jnp.arange(MOBA_BLOCK)

    def query_block(c):
        start = c * MOBA_Q_BLOCK
        t = start + jnp.arange(MOBA_Q_BLOCK)
        cb = start // MOBA_BLOCK
        qc = lax.dynamic_slice_in_dim(q, start, MOBA_Q_BLOCK, axis=3)
        sg = jnp.einsum('bgrqd,bgnd->bgrqn', qc, km).astype(jnp.float32)
        sg = jnp.where(blk_ids < cb, sg, -jnp.inf)
        _, idx = lax.top_k(sg, ktop)
        k_g = kb[bi, gi, idx]
        v_g = vb[bi, gi, idx]
        pos = idx[..., None] * MOBA_BLOCK + offs
        dist = (t[:, None, None] - pos).astype(jnp.float32)
        s_sel = jnp.einsum('bgrqd,bgrqkld->bgrqkl', qc, k_g).astype(jnp.float32)
        s_sel = jnp.where((idx < cb)[..., None], s_sel - slopes[..., None] * dist, -jnp.inf)
        ko = lax.dynamic_index_in_dim(kb, cb, axis=2, keepdims=False)
        vo = lax.dynamic_index_in_dim(vb, cb, axis=2, keepdims=False)
        dist_o = t[:, None] - (cb * MOBA_BLOCK + offs)[None, :]
        s_own = jnp.einsum('bgrqd,bgld->bgrql', qc, ko).astype(jnp.float32)
        s_own = jnp.where(dist_o >= 0, s_own - slopes * dist_o.astype(jnp.float32), -jnp.inf)
        n_sel_keys = ktop * MOBA_BLOCK
        sc = jnp.concatenate([s_sel.reshape(b, g, r, MOBA_Q_BLOCK, n_sel_keys), s_own], axis=-1)
        pr = jax.nn.softmax(sc, axis=-1)
        p_sel = pr[..., :n_sel_keys].reshape(s_sel.shape).astype(vb.dtype)
        p_own = pr[..., n_sel_keys:].astype(vb.dtype)
        o = (jnp.einsum('bgrqkl,bgrqkld->bgrqd', p_sel, v_g)
             + jnp.einsum('bgrql,bgld->bgrqd', p_own, vo))
        return o.transpose(0, 3, 1, 2, 4).reshape(b, MOBA_Q_BLOCK, ATT_DIM)

    o = lax.map(query_block, jnp.arange(s // MOBA_Q_BLOCK))
    o = o.transpose(1, 0, 2, 3).reshape(b, s, ATT_DIM)
    return o @ w_out


def hier_moe(xn, w_group, b_group, w_expert, b_expert, w_gate, w_up, w_down):
    b, s, d = xn.shape
    xt = xn.reshape(b * s, d)
    g_prob = jax.nn.softmax((xt @ w_group + b_group).astype(jnp.float32), axis=-1)
    g_w, g_idx = lax.top_k(g_prob, 1)
    e_logits = (xt @ w_expert + b_expert).astype(jnp.float32).reshape(-1, N_GROUPS, EXPERTS_PER_GROUP)
    e_logits = jnp.einsum('tge,tg->te', e_logits, jax.nn.one_hot(g_idx[:, 0], N_GROUPS, dtype=jnp.float32))
    e_prob = jax.nn.softmax(e_logits, axis=-1)
    e_w, e_idx = lax.top_k(e_prob, TOPK_IN_GROUP)
    e_w = e_w / jnp.sum(e_w, axis=-1, keepdims=True)
    comb = g_w * e_w
    expert_id = g_idx * EXPERTS_PER_GROUP + e_idx
    cw = jnp.einsum('tk,tke->te', comb, jax.nn.one_hot(expert_id, N_EXPERTS, dtype=jnp.float32)).astype(xt.dtype)
    hid = jax.nn.silu(jnp.einsum('td,edf->tef', xt, w_gate)) * jnp.einsum('td,edf->tef', xt, w_up)
    y = jnp.einsum('tef,efd->td', hid * cw[:, :, None], w_down)
    return y.reshape(b, s, d)


def setup_inputs(seed: int = 0) -> dict:
    key = jax.random.key(seed)
    keys = iter(jax.random.split(key, 40))

    def nrm(shape, scale):
        return jax.random.normal(next(keys), shape, jnp.float32) * scale

    def gain(shape):
        return 1.0 + 0.05 * jax.random.normal(next(keys), shape, jnp.float32)

    n_a = (DEPTH + 1) // 2
    n_b = DEPTH - n_a
    cmp_in = CMP_BLOCK * HEAD_DIM
    return {
        'x': nrm((BATCH, SEQ, D_MODEL), 1.0),
        'p': nrm((DEPTH, BATCH, SEQ, PLE_DIM), 1.0),
        'ln_mix': gain((DEPTH, D_MODEL)),
        'ln_ffn': gain((DEPTH, D_MODEL)),
        'ln_ple': gain((DEPTH, D_MODEL)),
        'a_w_in': nrm((n_a, D_MODEL, NSA_PROJ), D_MODEL ** -0.5),
        'a_q_norm': gain((n_a, HEAD_DIM)),
        'a_k_norm': gain((n_a, 3, HEAD_DIM)),
        'a_ck_pos': nrm((n_a, CMP_BLOCK, HEAD_DIM), 0.1),
        'a_ck_w1': nrm((n_a, cmp_in, CMP_HIDDEN), cmp_in ** -0.5),
        'a_ck_w2': nrm((n_a, CMP_HIDDEN, HEAD_DIM), CMP_HIDDEN ** -0.5),
        'a_cv_pos': nrm((n_a, CMP_BLOCK, HEAD_DIM), 0.1),
        'a_cv_w1': nrm((n_a, cmp_in, CMP_HIDDEN), cmp_in ** -0.5),
        'a_cv_w2': nrm((n_a, CMP_HIDDEN, HEAD_DIM), CMP_HIDDEN ** -0.5),
        'a_w_out': nrm((n_a, ATT_DIM, D_MODEL), ATT_DIM ** -0.5),
        'kv_norm': gain((D_MODEL,)),
        'w_kv_shared': nrm((D_MODEL, 2 * MOBA_KV_DIM), D_MODEL ** -0.5),
        'k_norm_shared': gain((HEAD_DIM,)),
        'b_w_q': nrm((n_b, D_MODEL, ATT_DIM), D_MODEL ** -0.5),
        'b_q_norm': gain((n_b, HEAD_DIM)),
        'b_w_out': nrm((n_b, ATT_DIM, D_MODEL), ATT_DIM ** -0.5),
        'moe_w_group': nrm((DEPTH, D_MODEL, N_GROUPS), D_MODEL ** -0.5),
        'moe_b_group': nrm((DEPTH, N_GROUPS), 0.01),
        'moe_w_expert': nrm((DEPTH, D_MODEL, N_EXPERTS), D_MODEL ** -0.5),
        'moe_b_expert': nrm((DEPTH, N_EXPERTS), 0.01),
        'moe_w_gate': nrm((DEPTH, N_EXPERTS, D_MODEL, D_EXPERT), D_MODEL ** -0.5),
        'moe_w_up': nrm((DEPTH, N_EXPERTS, D_MODEL, D_EXPERT), D_MODEL ** -0.5),
        'moe_w_down': nrm((DEPTH, N_EXPERTS, D_EXPERT, D_MODEL), D_EXPERT ** -0.5),
        'ple_w_proj': nrm((DEPTH, PLE_DIM, D_MODEL), PLE_DIM ** -0.5),
        'ple_w_gate': nrm((DEPTH, D_MODEL, D_MODEL), D_MODEL ** -0.5),
    }


def reference(x, p, ln_mix, ln_ffn, ln_ple, a_w_in, a_q_norm, a_k_norm, a_ck_pos, a_ck_w1, a_ck_w2,
              a_cv_pos, a_cv_w1, a_cv_w2, a_w_out, kv_norm, w_kv_shared, k_norm_shared, b_w_q, b_q_norm,
              b_w_out, moe_w_group, moe_b_group, moe_w_expert, moe_b_expert, moe_w_gate, moe_w_up,
              moe_w_down, ple_w_proj, ple_w_gate):
    n_a = (DEPTH + 1) // 2
    h = x
    kb = vb = km = None
    for i in range(DEPTH):
        xn = rms_norm(h, ln_mix[i])
        if i < n_a:
            mix = nsa_attention(xn, a_w_in[i], a_q_norm[i], a_k_norm[i], a_ck_pos[i], a_ck_w1[i], a_ck_w2[i],
                                a_cv_pos[i], a_cv_w1[i], a_cv_w2[i], a_w_out[i])
        else:
            if i == n_a:
                kb, vb, km = shared_kv(h, kv_norm, w_kv_shared, k_norm_shared)
            j = i - n_a
            mix = moba_attention(xn, kb, vb, km, b_w_q[j], b_q_norm[j], b_w_out[j])
        h = h + mix.astype(h.dtype)
        h = h + hier_moe(rms_norm(h, ln_ffn[i]), moe_w_group[i], moe_b_group[i], moe_w_expert[i],
                         moe_b_expert[i], moe_w_gate[i], moe_w_up[i], moe_w_down[i]).astype(h.dtype)
        gate = jax.nn.sigmoid(rms_norm(h, ln_ple[i]) @ ple_w_gate[i])
        h = h + (gate * (p[i] @ ple_w_proj[i])).astype(h.dtype)
    return h
```

```python
from contextlib import ExitStack
import numpy as np
import concourse.bass as bass
import concourse.mybir as mybir


F32 = mybir.dt.float32
BF16 = mybir.dt.bfloat16
I32 = mybir.dt.int32
U32 = mybir.dt.uint32
ALU = mybir.AluOpType
AF = mybir.ActivationFunctionType
AX = mybir.AxisListType

ENGS = ("pe", "act", "dve", "pool", "sp")
N_DMA_SEMS = {"sp": 20, "pool": 6, "act": 4}


class Buf:
    __slots__ = ("name", "w", "r")

    def __init__(self, name="b"):
        self.name = name
        self.w = None
        self.r = {}


class Prog:
    def __init__(self, nc, stack):
        self.nc = nc
        self.q = {e: [] for e in ENGS}
        self.cnt = {}
        self.sems = {}
        self.seen = {e: {} for e in ENGS}
        for e in ("pe", "act", "dve", "pool"):
            k = "c_" + e
            self.sems[k] = stack.enter_context(nc.semaphore(k))
            self.cnt[k] = 0
        self.dma_pool = {}
        self.dma_rr = {}
        for e, n in N_DMA_SEMS.items():
            ks = []
            for i in range(n):
                k = "d_%s_%d" % (e, i)
                self.sems[k] = stack.enter_context(nc.semaphore(k))
                self.cnt[k] = 0
                ks.append(k)
            self.dma_pool[e] = ks
            self.dma_rr[e] = 0

    def _need(self, eng, deps):
        out = []
        seen = self.seen[eng]
        best = {}
        for d in deps:
            if d is None:
                continue
            k, v = d
            if best.get(k, 0) < v:
                best[k] = v
        for k, v in best.items():
            if seen.get(k, 0) < v:
                seen[k] = v
                out.append((k, v))
        return out

    def _deps(self, eng, reads, writes, own_key):
        deps = []
        for b in reads:
            deps.append(b.w)
        for b in writes:
            deps.append(b.w)
            for k, v in b.r.items():
                deps.append((k, v))
        if eng == "pe":
            deps = [d for d in deps if d is not None and d[0] != "c_pe"]
        return deps

    def op(self, eng, fn, reads=(), writes=()):
        key = "c_" + eng
        waits = self._need(eng, self._deps(eng, reads, writes, key))
        self.cnt[key] += 1
        val = self.cnt[key]
        self.q[eng].append((waits, fn, key, 1))
        for b in reads:
            if b.r.get(key, 0) < val:
                b.r[key] = val
        for b in writes:
            b.w = (key, val)
            b.r = {}
        return (key, val)

    def dma(self, eng, out_ap, in_ap, reads=(), writes=(), **kw):
        pool = self.dma_pool[eng]
        key = pool[self.dma_rr[eng] % len(pool)]
        self.dma_rr[eng] += 1
        deps = self._deps(eng, reads, writes, key)
        if self.cnt[key] > 0:
            deps.append((key, self.cnt[key]))
        waits = self._need(eng, deps)
        self.cnt[key] += 16
        val = self.cnt[key]

        def fn(e, out_ap=out_ap, in_ap=in_ap, kw=kw):
            return e.dma_start(out=out_ap, in_=in_ap, **kw)
        self.q[eng].append((waits, fn, key, 16))
        for b in reads:
            if b.r.get(key, 0) < val:
                b.r[key] = val
        for b in writes:
            b.w = (key, val)
            b.r = {}
        return (key, val)

    def barrier(self):
        allv = [(k, v) for k, v in self.cnt.items() if v > 0]
        for e in ENGS:
            waits = self._need(e, allv)
            if waits:
                self.q[e].append((waits, None, None, 0))

    def emit(self):
        nc = self.nc
        with nc.Block() as block:
            def run(engname):
                def body(e):
                    for waits, fn, key, inc in self.q[engname]:
                        for k, v in waits:
                            e.wait_ge(self.sems[k], v)
                        if fn is not None:
                            ins = fn(e)
                            ins.then_inc(self.sems[key], inc)
                return body
            block.tensor(run("pe"))
            block.scalar(run("act"))
            block.vector(run("dve"))
            block.gpsimd(run("pool"))
            block.sync(run("sp"))


class Tl:
    def __init__(self, t, name, nsub=0):
        self.t = t
        self.b = Buf(name)
        self.bs = [Buf("%s_%d" % (name, i)) for i in range(nsub)]

    def __getitem__(self, k):
        return self.t[k]


def _bufs(xs):
    out = []
    for x in xs:
        if isinstance(x, Tl):
            out.append(x.b)
        elif isinstance(x, Buf):
            out.append(x)
        elif isinstance(x, (list, tuple)):
            out.extend(_bufs(x))
        else:
            raise TypeError(x)
    return out


class Ctx:
    def __init__(self, nc, P):
        self.nc = nc
        self.P = P
        self.uid = 0

    def sb(self, st, shape, dt, name, nsub=0):
        self.uid += 1
        nm = "%s_%d" % (name, self.uid)
        return Tl(st.enter_context(self.nc.sbuf_tensor(nm, shape, dt)), nm, nsub)

    def ps(self, st, shape, dt, name, nsub=0):
        self.uid += 1
        nm = "%s_%d" % (name, self.uid)
        return Tl(st.enter_context(self.nc.psum_tensor(nm, shape, dt)), nm, nsub)

    def op(self, eng, fn, r=(), w=()):
        return self.P.op(eng, fn, _bufs(r), _bufs(w))

    def dma(self, eng, out_ap, in_ap, r=(), w=(), **kw):
        return self.P.dma(eng, out_ap, in_ap, _bufs(r), _bufs(w), **kw)


def _prog_custom16(self, eng, fn, reads=(), writes=()):
    pool = self.dma_pool[eng]
    key = pool[self.dma_rr[eng] % len(pool)]
    self.dma_rr[eng] += 1
    deps = self._deps(eng, reads, writes, key)
    if self.cnt[key] > 0:
        deps.append((key, self.cnt[key]))
    waits = self._need(eng, deps)
    self.cnt[key] += 16
    val = self.cnt[key]
    self.q[eng].append((waits, fn, key, 16))
    for b in reads:
        if b.r.get(key, 0) < val:
            b.r[key] = val
    for b in writes:
        b.w = (key, val)
        b.r = {}
    return (key, val)


Prog.custom16 = _prog_custom16


def rms_stats(C, src_ap, src_bufs, junk, ss, rstd, mhalf, width):
    C.op("act", lambda e: e.activation(out=junk[:, 0:width], in_=src_ap, func=AF.Square),
         r=src_bufs, w=[junk])
    C.op("dve", lambda e: e.reduce_sum(out=ss[:], in_=junk[:, 0:width], axis=AX.X), r=[junk], w=[ss])
    C.op("dve", lambda e: e.tensor_scalar(out=ss[:], in0=ss[:], scalar1=1.0 / width, scalar2=1e-6,
                                          op0=ALU.mult, op1=ALU.add), r=[ss], w=[ss])
    C.op("act", lambda e: e.activation(out=ss[:], in_=ss[:], func=AF.Sqrt), r=[ss], w=[ss])
    C.op("dve", lambda e: e.reciprocal(out=rstd[:], in_=ss[:]), r=[ss], w=[rstd])


def phase_B(C, NT, d):
    nc = C.nc
    NTT = NT // 128
    CH = 256
    NCH = NT // CH
    with ExitStack() as st:
        hres = C.sb(st, [128, NTT, 1024], F32, "hres", nsub=NTT)
        xn2T = C.sb(st, [128, 8, NT], BF16, "xn2T", nsub=NTT)
        cwT = C.sb(st, [32, NT], BF16, "cwT", nsub=NTT)
        identb = C.sb(st, [128, 128], BF16, "identb")
        identf = C.sb(st, [128, 128], F32, "identf")
        gffn = C.sb(st, [128, 1024], F32, "gffn")
        gple = C.sb(st, [128, 1024], F32, "gple")
        mhalf = C.sb(st, [128, 1], F32, "mhalf")
        sele = C.sb(st, [32, 32, 128], BF16, "sele")
        C.dma("sp", identb[:], d["identb"], w=[identb])
        C.dma("sp", identf[:], d["identf"], w=[identf])
        C.dma("sp", sele[:], d["sele"], w=[sele])
        C.dma("sp", gffn[:], d["ln_ffn"].partition_broadcast(128), w=[gffn])
        C.dma("sp", gple[:], d["ln_ple"].partition_broadcast(128), w=[gple])
        C.op("pool", lambda e: e.memset(mhalf[:], -0.5), w=[mhalf])

        with ExitStack() as s1:
            wst = [C.sb(s1, [128, 1024], F32, "wst%d" % i) for i in range(2)]
            wo = C.sb(s1, [128, 8, 1024], BF16, "wo", nsub=8)
            wr = C.sb(s1, [128, 8, 36], F32, "wr")
            rbias = C.sb(s1, [128, 36], F32, "rbias")
            ot = [C.sb(s1, [128, 1024], BF16, "ot%d" % i) for i in range(2)]
            oT = C.sb(s1, [128, 8, 128], BF16, "oT")
            junk = C.sb(s1, [128, 1024], F32, "junk")
            xn2 = C.sb(s1, [128, 1024], F32, "xn2")
            xn2b = C.sb(s1, [128, 1024], BF16, "xn2b")
            xfT = C.sb(s1, [128, 8, 128], F32, "xfT")
            ss = C.sb(s1, [128, 1], F32, "ss")
            rstd = C.sb(s1, [128, 1], F32, "rstd")
            lg = C.sb(s1, [128, 36], F32, "lg")
            sm = C.sb(s1, [128, 64], F32, "sm")
            cw = C.sb(s1, [128, 32], BF16, "cw")
            pT = C.ps(s1, [128, 8, 128], BF16, "pT")
            pm = [C.ps(s1, [128, 512], F32, "pm%d" % i) for i in range(2)]
            pTf = C.ps(s1, [128, 8, 128], F32, "pTf")
            plg = C.ps(s1, [128, 36], F32, "plg")
            pcw = C.ps(s1, [32, 128], BF16, "pcw")

            for c in range(8):
                C.dma("sp", wst[c % 2][:], d["w_out"][c * 128:(c + 1) * 128, :], w=[wst[c % 2]])
                C.op("pool", lambda e, c=c: e.tensor_copy(out=wo[:, c, :], in_=wst[c % 2][:]),
                     r=[wst[c % 2]], w=[wo.bs[c]])
            C.dma("sp", wr[:, :, 0:4], d["w_group"].rearrange("(c p) n -> p c n", p=128), w=[wr])
            C.dma("sp", wr[:, :, 4:36], d["w_expert"].rearrange("(c p) n -> p c n", p=128), w=[wr])
            C.dma("sp", rbias[:, 0:4], d["b_group"].partition_broadcast(128), w=[rbias])
            C.dma("sp", rbias[:, 4:36], d["b_expert"].partition_broadcast(128), w=[rbias])

            for t in range(NTT):
                tk = slice(t * 128, (t + 1) * 128)
                hb = hres.bs[t]
                o_t = ot[t % 2]
                C.dma("sp", hres[:, t, :], d["hin"][tk, :], w=[hb])
                C.dma("sp", o_t[:], d["o"][tk, :], w=[o_t])
                for c in range(8):
                    C.op("pe", lambda e, c=c, o_t=o_t: e.transpose(out=pT[:, c, :], in_=o_t[:, c * 128:(c + 1) * 128],
                                                                    identity=identb[:]), r=[o_t, identb], w=[pT])
                C.op("act", lambda e: e.activation(out=oT[:], in_=pT[:], func=AF.Copy), r=[pT], w=[oT])
                for hf in range(2):
                    for c in range(8):
                        C.op("pe", lambda e, c=c, hf=hf: e.matmul(pm[hf][:], lhsT=oT[:, c, :],
                                                                   rhs=wo[:, c, hf * 512:(hf + 1) * 512],
                                                                   start=(c == 0), stop=(c == 7)),
                             r=[oT, wo.bs[c]], w=[pm[hf]])
                for hf in range(2):
                    C.op("dve", lambda e, hf=hf, t=t: e.tensor_tensor(out=hres[:, t, hf * 512:(hf + 1) * 512],
                                                                       in0=pm[hf][:],
                                                                       in1=hres[:, t, hf * 512:(hf + 1) * 512],
                                                                       op=ALU.add), r=[pm[hf], hb], w=[hb])
                rms_stats(C, hres[:, t, :], [hb], junk, ss, rstd, mhalf, 1024)
                C.op("dve", lambda e, t=t: e.scalar_tensor_tensor(out=xn2[:], in0=hres[:, t, :], scalar=rstd[:, 0:1],
                                                                   in1=gffn[:], op0=ALU.mult, op1=ALU.mult),
                     r=[hb, rstd, gffn], w=[xn2])
                if "dbg7" in d and t == 0:
                    C.dma("sp", d["dbg7"][0], xn2[:], r=[xn2])
                    C.op("act", lambda e, t=t: e.activation(out=junk[:], in_=hres[:, t, :], func=AF.Copy, scale=rstd[:, 0:1]), r=[hb, rstd], w=[junk])
                    C.dma("sp", d["dbg7"][1], junk[:], r=[junk])
                    C.op("dve", lambda e, t=t: e.tensor_scalar(out=junk[:], in0=hres[:, t, :], scalar1=rstd[:, 0:1], scalar2=None, op0=ALU.mult), r=[hb, rstd], w=[junk])
                    C.dma("sp", d["dbg7"][2], junk[:], r=[junk])
                    C.dma("sp", d["dbg7"][3], hres[:, t, :], r=[hb])
                C.op("act", lambda e: e.activation(out=xn2b[:], in_=xn2[:], func=AF.Copy), r=[xn2], w=[xn2b])
                for c in range(8):
                    C.op("pe", lambda e, c=c: e.transpose(out=pT[:, c, :], in_=xn2b[:, c * 128:(c + 1) * 128],
                                                           identity=identb[:]), r=[xn2b, identb], w=[pT])
                C.op("dve", lambda e, tk=tk: e.tensor_copy(out=xn2T[:, :, tk], in_=pT[:]), r=[pT], w=[xn2T.bs[t]])
                if "dbg4" in d and t == 0:
                    C.dma("sp", d["dbg5"][:, 0:1], rstd[:], r=[rstd], allow_slow_non_contiguous=True)
                    C.dma("sp", d["dbg5"][:, 1:2], ss[:], r=[ss], allow_slow_non_contiguous=True)
                    C.dma("sp", d["dbg6"][0], junk[:], r=[junk])
                    C.dma("sp", d["dbg6"][1], xn2[:], r=[xn2])
                    C.dma("sp", d["dbg6"][2], gffn[:], r=[gffn])
                    C.dma("sp", d["dbg4"][0], xn2b[:], r=[xn2b])
                    for c in range(8):
                        C.dma("sp", d["dbg4"][1][:, c * 128:(c + 1) * 128], xn2T[:, c, tk], r=[xn2T.bs[t]])
                for c in range(8):
                    C.op("pe", lambda e, c=c: e.transpose(out=pTf[:, c, :], in_=xn2[:, c * 128:(c + 1) * 128],
                                                           identity=identf[:]), r=[xn2, identf], w=[pTf])
                C.op("act", lambda e: e.activation(out=xfT[:], in_=pTf[:], func=AF.Copy), r=[pTf], w=[xfT])
                for c in range(8):
                    C.op("pe", lambda e, c=c: e.matmul(plg[:], lhsT=xfT[:, c, :], rhs=wr[:, c, :],
                                                        start=(c == 0), stop=(c == 7)), r=[xfT, wr], w=[plg])
                C.op("dve", lambda e: e.tensor_tensor(out=lg[:], in0=plg[:], in1=rbias[:], op=ALU.add),
                     r=[plg, rbias], w=[lg])
                gmax, ngmax, gsum, gw = sm[:, 0:1], sm[:, 1:2], sm[:, 2:3], sm[:, 3:4]
                oh, ge = sm[:, 4:8], sm[:, 8:12]
                sel, m8 = sm[:, 16:24], sm[:, 24:32]
                nl1, e2, w1, c1, c2 = sm[:, 32:33], sm[:, 33:34], sm[:, 34:35], sm[:, 35:36], sm[:, 36:37]
                ta, tb = sm[:, 40:48], sm[:, 48:56]
                D = lambda fn, r=(lg, sm), w=(sm,): C.op("dve", fn, r=list(r), w=list(w))
                D(lambda e: e.reduce_max(out=gmax, in_=lg[:, 0:4], axis=AX.X))
                D(lambda e: e.tensor_scalar(out=ngmax, in0=gmax, scalar1=-1.0, scalar2=None, op0=ALU.mult))
                C.op("act", lambda e: e.activation(out=ge, in_=lg[:, 0:4], func=AF.Exp, bias=ngmax, scale=1.0),
                     r=[lg, sm], w=[sm])
                D(lambda e: e.reduce_sum(out=gsum, in_=ge, axis=AX.X))
                D(lambda e: e.reciprocal(out=gw, in_=gsum))
                D(lambda e: e.tensor_scalar(out=oh, in0=lg[:, 0:4], scalar1=gmax, scalar2=None, op0=ALU.is_equal))
                D(lambda e: e.tensor_scalar(out=sel, in0=lg[:, 4:12], scalar1=oh[:, 0:1], scalar2=None, op0=ALU.mult))
                for g in range(1, 4):
                    D(lambda e, g=g: e.scalar_tensor_tensor(out=sel, in0=lg[:, 4 + 8 * g:12 + 8 * g],
                                                             scalar=oh[:, g:g + 1], in1=sel,
                                                             op0=ALU.mult, op1=ALU.add))
                D(lambda e: e.max(out=m8, in_=sel))
                D(lambda e: e.tensor_scalar(out=nl1, in0=m8[:, 0:1], scalar1=-1.0, scalar2=None, op0=ALU.mult))
                C.op("act", lambda e: e.activation(out=e2, in_=m8[:, 1:2], func=AF.Exp, bias=nl1, scale=1.0),
                     r=[sm], w=[sm])
                D(lambda e: e.tensor_scalar(out=w1, in0=e2, scalar1=1.0, scalar2=None, op0=ALU.add))
                D(lambda e: e.reciprocal(out=w1, in_=w1))
                D(lambda e: e.tensor_tensor(out=c1, in0=w1, in1=gw, op=ALU.mult))
                D(lambda e: e.tensor_tensor(out=c2, in0=c1, in1=e2, op=ALU.mult))
                D(lambda e: e.tensor_scalar(out=ta, in0=sel, scalar1=m8[:, 0:1], scalar2=c1, op0=ALU.is_equal,
                                            op1=ALU.mult))
                D(lambda e: e.tensor_scalar(out=tb, in0=sel, scalar1=m8[:, 1:2], scalar2=c2, op0=ALU.is_equal,
                                            op1=ALU.mult))
                D(lambda e: e.tensor_tensor(out=ta, in0=ta, in1=tb, op=ALU.add))
                for g in range(4):
                    C.op("dve", lambda e, g=g: e.tensor_scalar(out=cw[:, 8 * g:8 * g + 8], in0=ta,
                                                                scalar1=oh[:, g:g + 1], scalar2=None, op0=ALU.mult),
                         r=[sm], w=[cw])
                C.op("pe", lambda e: e.transpose(out=pcw[:], in_=cw[:], identity=identb[:]), r=[cw, identb], w=[pcw])
                C.op("act", lambda e, tk=tk: e.activation(out=cwT[:, tk], in_=pcw[:], func=AF.Copy),
                     r=[pcw], w=[cwT.bs[t]])
        C.P.barrier()
        if "dbg1" in d:
            for t in range(NTT):
                C.dma("sp", d["dbg1"][t * 128:(t + 1) * 128, :], hres[:, t, :], r=[hres.bs[t]])
            C.dma("sp", d["dbgcw"], cwT[:], r=cwT.bs)

        with ExitStack() as s2:
            NST = 3
            stg = [C.sb(s2, [128, 8, 128], F32, "stg%d" % i) for i in range(NST)]
            GS = 4
            wg = [[C.sb(s2, [128, 8, 128], BF16, "wg%d_%d" % (i, j)) for j in range(GS)] for i in range(2)]
            wu = [[C.sb(s2, [128, 8, 128], BF16, "wu%d_%d" % (i, j)) for j in range(GS)] for i in range(2)]
            wd = [[C.sb(s2, [128, 1024], BF16, "wd%d_%d" % (i, j)) for j in range(GS)] for i in range(2)]
            sg = [C.sb(s2, [128, CH], BF16, "sg%d" % i) for i in range(2)]
            bcs = [C.sb(s2, [128, CH], BF16, "bcs%d" % i) for i in range(2)]
            tt = [C.sb(s2, [128, CH], BF16, "tt%d" % i) for i in range(2)]
            hid = [C.sb(s2, [128, CH], BF16, "hid%d" % i) for i in range(2)]
            pgu = [C.ps(s2, [128, 2, CH], F32, "pgu%d" % i) for i in range(2)]
            pbc = [C.ps(s2, [128, CH], F32, "pbc%d" % i) for i in range(2)]
            py = [[C.ps(s2, [128, 512], F32, "py%d_%d" % (s, hf)) for hf in range(2)] for s in range(2)]
            nst = [0]

            def load_group(G):
                par = G % 2
                for j in range(GS):
                    ex = G * GS + j
                    for (dst, src, is_d) in ((wg[par][j], d["w_gate"], 0), (wu[par][j], d["w_up"], 0),
                                             (wd[par][j], d["w_down"], 1)):
                        s_t = stg[nst[0] % NST]
                        nst[0] += 1
                        if is_d:
                            sv = s_t[:].rearrange("p c f -> p (c f)")
                            C.dma("sp", sv, src[ex], w=[s_t])
                            C.op("pool", lambda e, dst=dst, sv=sv: e.tensor_copy(out=dst[:], in_=sv), r=[s_t], w=[dst])
                        else:
                            C.dma("sp", s_t[:], src[ex].rearrange("(c p) f -> p c f", p=128), w=[s_t])
                            C.op("pool", lambda e, dst=dst, s_t=s_t: e.tensor_copy(out=dst[:], in_=s_t[:]),
                                 r=[s_t], w=[dst])

            def GU(G, ck, j, k):
                par = G % 2
                ex = G * GS + j
                cs = slice(ck * CH, (ck + 1) * CH)
                tb = [xn2T.bs[ck * (CH // 128) + i] for i in range(CH // 128)]
                for (which, wt) in ((0, wg[par][j]), (1, wu[par][j])):
                    for c in range(8):
                        C.op("pe", lambda e, c=c, wt=wt, which=which: e.matmul(pgu[k][:, which, :], lhsT=wt[:, c, :],
                                                                                rhs=xn2T[:, c, cs],
                                                                                start=(c == 0), stop=(c == 7)),
                             r=[wt] + tb, w=[pgu[k]])
                cb = [cwT.bs[ck * (CH // 128) + i] for i in range(CH // 128)]
                C.op("pe", lambda e: e.matmul(pbc[k][:], lhsT=sele[:, ex, :], rhs=cwT[:, cs], start=True, stop=True),
                     r=[sele] + cb, w=[pbc[k]])
                C.op("act", lambda e: e.activation(out=sg[k][:], in_=pgu[k][:, 0, :], func=AF.Silu),
                     r=[pgu[k]], w=[sg[k]])
                C.op("act", lambda e: e.activation(out=bcs[k][:], in_=pbc[k][:], func=AF.Copy), r=[pbc[k]], w=[bcs[k]])
                C.op("dve", lambda e: e.tensor_tensor(out=tt[k][:], in0=pgu[k][:, 1, :], in1=sg[k][:], op=ALU.mult),
                     r=[pgu[k], sg[k]], w=[tt[k]])
                C.op("pool", lambda e: e.tensor_tensor(out=hid[k][:], in0=tt[k][:], in1=bcs[k][:], op=ALU.mult),
                     r=[tt[k], bcs[k]], w=[hid[k]])
                if "dbg3" in d and G == 0 and ck == 0 and j == 1:
                    for i, tl in enumerate((sg[k], bcs[k], tt[k], hid[k])):
                        C.dma("sp", d["dbg3"][i], tl[:], r=[tl])
                    C.dma("sp", d["dbg3"][4][:, 0:128], wg[par][j][:, 3, :], r=[wg[par][j]])
                    C.dma("sp", d["dbg3"][5][:, 0:128], xn2T[:, 3, 0:128], r=tb)

            def DOWN(G, ck, j, k):
                par = G % 2
                for s in range(CH // 128):
                    for hf in range(2):
                        C.op("pe", lambda e, s=s, hf=hf: e.matmul(py[s][hf][:], lhsT=hid[k][:, s * 128:(s + 1) * 128],
                                                                   rhs=wd[par][j][:, hf * 512:(hf + 1) * 512],
                                                                   start=(j == 0), stop=(j == GS - 1)),
                             r=[hid[k], wd[par][j]], w=[py[s][hf]])

            NG = 32 // GS
            load_group(0)
            for G in range(NG):
                if G + 1 < NG:
                    load_group(G + 1)
                for ck in range(NCH):
                    GU(G, ck, 0, 0)
                    for j in range(1, GS):
                        GU(G, ck, j, j % 2)
                        DOWN(G, ck, j - 1, (j - 1) % 2)
                    DOWN(G, ck, GS - 1, (GS - 1) % 2)
                    for s in range(CH // 128):
                        t = ck * (CH // 128) + s
                        for hf in range(2):
                            C.op("dve", lambda e, s=s, hf=hf, t=t: e.tensor_tensor(
                                out=hres[:, t, hf * 512:(hf + 1) * 512], in0=py[s][hf][:],
                                in1=hres[:, t, hf * 512:(hf + 1) * 512], op=ALU.add),
                                r=[py[s][hf], hres.bs[t]], w=[hres.bs[t]])
        C.P.barrier()
        if "dbg2" in d:
            for t in range(NTT):
                C.dma("sp", d["dbg2"][t * 128:(t + 1) * 128, :], hres[:, t, :], r=[hres.bs[t]])

        with ExitStack() as s3:
            wst_3 = [C.sb(s3, [128, 1024], F32, "wst3_%d" % i) for i in range(2)]
            wpg = C.sb(s3, [128, 8, 1024], BF16, "wpg", nsub=8)
            wpp = C.sb(s3, [128, 2, 1024], BF16, "wpp", nsub=2)
            junk_3 = C.sb(s3, [128, 1024], F32, "junk3")
            ss_3 = C.sb(s3, [128, 1], F32, "ss3")
            rstd_3 = C.sb(s3, [128, 1], F32, "rstd3")
            xn3 = C.sb(s3, [128, 1024], BF16, "xn3")
            x3T = C.sb(s3, [128, 8, 128], BF16, "x3T")
            pt = [C.sb(s3, [128, 256], F32, "pt%d" % i) for i in range(2)]
            ptb = C.sb(s3, [128, 256], BF16, "ptb")
            ppT = C.sb(s3, [128, 2, 128], BF16, "ppT")
            sig = C.sb(s3, [128, 1024], F32, "sig")
            gp = C.sb(s3, [128, 1024], F32, "gp")
            ho = [C.sb(s3, [128, 1024], F32, "ho%d" % i) for i in range(2)]
            pT_3 = C.ps(s3, [128, 8, 128], BF16, "pT3")
            pT2 = C.ps(s3, [128, 2, 128], BF16, "pT23")
            pg = [C.ps(s3, [128, 512], F32, "pg%d" % i) for i in range(2)]
            pq = [C.ps(s3, [128, 512], F32, "pq%d" % i) for i in range(2)]
            for c in range(8):
                C.dma("sp", wst_3[c % 2][:], d["ple_w_gate"][c * 128:(c + 1) * 128, :], w=[wst_3[c % 2]])
                C.op("pool", lambda e, c=c: e.tensor_copy(out=wpg[:, c, :], in_=wst_3[c % 2][:]),
                     r=[wst_3[c % 2]], w=[wpg.bs[c]])
            for c in range(2):
                C.dma("sp", wst_3[c % 2][:], d["ple_w_proj"][c * 128:(c + 1) * 128, :], w=[wst_3[c % 2]])
                C.op("pool", lambda e, c=c: e.tensor_copy(out=wpp[:, c, :], in_=wst_3[c % 2][:]),
                     r=[wst_3[c % 2]], w=[wpp.bs[c]])
            for t in range(NTT):
                tk = slice(t * 128, (t + 1) * 128)
                hb = hres.bs[t]
                p_t = pt[t % 2]
                h_o = ho[t % 2]
                C.dma("sp", p_t[:], d["p"][tk, :], w=[p_t])
                rms_stats(C, hres[:, t, :], [hb], junk_3, ss_3, rstd_3, mhalf, 1024)
                C.op("dve", lambda e, t=t: e.scalar_tensor_tensor(out=xn3[:], in0=hres[:, t, :], scalar=rstd_3[:, 0:1],
                                                                   in1=gple[:], op0=ALU.mult, op1=ALU.mult),
                     r=[hb, rstd_3, gple], w=[xn3])
                for c in range(8):
                    C.op("pe", lambda e, c=c: e.transpose(out=pT_3[:, c, :], in_=xn3[:, c * 128:(c + 1) * 128],
                                                           identity=identb[:]), r=[xn3, identb], w=[pT_3])
                C.op("act", lambda e: e.activation(out=x3T[:], in_=pT_3[:], func=AF.Copy), r=[pT_3], w=[x3T])
                C.op("pool", lambda e, p_t=p_t: e.tensor_copy(out=ptb[:], in_=p_t[:]), r=[p_t], w=[ptb])
                for c in range(2):
                    C.op("pe", lambda e, c=c: e.transpose(out=pT2[:, c, :], in_=ptb[:, c * 128:(c + 1) * 128],
                                                           identity=identb[:]), r=[ptb, identb], w=[pT2])
                C.op("dve", lambda e: e.tensor_copy(out=ppT[:], in_=pT2[:]), r=[pT2], w=[ppT])
                for hf in range(2):
                    for c in range(8):
                        C.op("pe", lambda e, c=c, hf=hf: e.matmul(pg[hf][:], lhsT=x3T[:, c, :],
                                                                   rhs=wpg[:, c, hf * 512:(hf + 1) * 512],
                                                                   start=(c == 0), stop=(c == 7)),
                             r=[x3T, wpg.bs[c]], w=[pg[hf]])
                    for c in range(2):
                        C.op("pe", lambda e, c=c, hf=hf: e.matmul(pq[hf][:], lhsT=ppT[:, c, :],
                                                                   rhs=wpp[:, c, hf * 512:(hf + 1) * 512],
                                                                   start=(c == 0), stop=(c == 1)),
                             r=[ppT, wpp.bs[c]], w=[pq[hf]])
                for hf in range(2):
                    hs = slice(hf * 512, (hf + 1) * 512)
                    C.op("act", lambda e, hf=hf, hs=hs: e.activation(out=sig[:, hs], in_=pg[hf][:], func=AF.Sigmoid),
                         r=[pg[hf]], w=[sig])
                    C.op("dve", lambda e, hf=hf, hs=hs: e.tensor_tensor(out=gp[:, hs], in0=pq[hf][:], in1=sig[:, hs],
                                                                         op=ALU.mult), r=[pq[hf], sig], w=[gp])
                C.op("pool", lambda e, t=t, h_o=h_o: e.tensor_tensor(out=h_o[:], in0=gp[:], in1=hres[:, t, :],
                                                                      op=ALU.add), r=[gp, hb], w=[h_o])
                C.dma("sp", d["hout"][tk, :], h_o[:], r=[h_o])
        C.P.barrier()


NEG = -30000.0


def mm(C, out, lhsT, rhs, start, stop, r, w):
    C.op("pe", lambda e: e.matmul(out, lhsT=lhsT, rhs=rhs, start=start, stop=stop), r=r, w=w)


def tr(C, out, in_, ident, r, w):
    C.op("pe", lambda e: e.transpose(out=out, in_=in_, identity=ident), r=r, w=w)


def act(C, out, in_, func, r, w, bias=None, scale=1.0):
    if bias is None:
        C.op("act", lambda e: e.activation(out=out, in_=in_, func=func, scale=scale), r=r, w=w)
    else:
        C.op("act", lambda e: e.activation(out=out, in_=in_, func=func, bias=bias, scale=scale), r=r, w=w)


def tt(C, eng, out, in0, in1, op, r, w):
    C.op(eng, lambda e: e.tensor_tensor(out=out, in0=in0, in1=in1, op=op), r=r, w=w)


def ts(C, eng, out, in0, s1, s2, op0, op1, r, w):
    if s2 is None:
        C.op(eng, lambda e: e.tensor_scalar(out=out, in0=in0, scalar1=s1, scalar2=None, op0=op0), r=r, w=w)
    else:
        C.op(eng, lambda e: e.tensor_scalar(out=out, in0=in0, scalar1=s1, scalar2=s2, op0=op0, op1=op1), r=r, w=w)


def stt(C, out, in0, scalar, in1, op0, op1, r, w):
    C.op("dve", lambda e: e.scalar_tensor_tensor(out=out, in0=in0, scalar=scalar, in1=in1, op0=op0, op1=op1),
         r=r, w=w)


def cp(C, eng, out, in_, r, w):
    C.op(eng, lambda e: e.tensor_copy(out=out, in_=in_), r=r, w=w)


def memset(C, eng, ap, val, w):
    C.op(eng, lambda e: e.memset(ap, val), w=w)


def asel(C, out, in_, pattern, base, cm, r, w):
    C.op("pool", lambda e: e.affine_select(out=out, in_=in_, pattern=pattern, compare_op=ALU.is_ge, fill=0.0,
                                           base=base, channel_multiplier=cm), r=r, w=w)


def rsqrt_chain(C, ssq, n_inv, r, w):
    ts(C, "dve", ssq, ssq, n_inv, 1e-6, ALU.mult, ALU.add, r=r, w=w)
    act(C, ssq, ssq, AF.Sqrt, r=r, w=w)
    C.op("dve", lambda e: e.reciprocal(out=ssq, in_=ssq), r=r, w=w)


def headnorm(C, src, n, gain_t, scale, sq, ssq, outs, srcb):
    sqv = sq[:, 0:n * 64]
    act(C, sqv, src, AF.Square, r=[srcb], w=[sq])
    C.op("dve", lambda e: e.tensor_reduce(out=ssq[:, 0:n], in_=sq[:, 0:n * 64].rearrange("p (n d) -> p n d", d=64),
                                          axis=AX.X, op=ALU.add), r=[sq], w=[ssq])
    ts(C, "dve", ssq[:, 0:n], ssq[:, 0:n], 1.0 / 64, 1e-6, ALU.mult, ALU.add, r=[ssq], w=[ssq])
    act(C, ssq[:, 0:n], ssq[:, 0:n], AF.Sqrt, r=[ssq], w=[ssq])
    C.op("dve", lambda e: e.reciprocal(out=ssq[:, 0:n], in_=ssq[:, 0:n]), r=[ssq], w=[ssq])
    if scale != 1.0:
        ts(C, "dve", ssq[:, 0:n], ssq[:, 0:n], float(scale), None, ALU.mult, None, r=[ssq], w=[ssq])
    s3 = src.rearrange("p (n d) -> p n d", d=64)
    q3 = sq[:, 0:n * 64].rearrange("p (n d) -> p n d", d=64)
    rb = ssq[:, 0:n].unsqueeze(2).to_broadcast([128, n, 64])
    tt(C, "dve", q3, s3, rb, ALU.mult, r=[srcb, ssq, sq], w=[sq])
    g3 = gain_t[:, 0:n * 64].rearrange("p (n d) -> p n d", d=64)
    for (o_ap, o_t) in outs:
        tt(C, "dve", o_ap.rearrange("p (n d) -> p n d", d=64), q3, g3, ALU.mult, r=[sq, gain_t], w=[o_t])


def prep(C, d, kind):
    nc = C.nc
    nsa = (kind == "nsa")
    NW = 1304 if nsa else 768
    with ExitStack() as st:
        identb = C.sb(st, [128, 128], BF16, "identb")
        identf = C.sb(st, [128, 128], F32, "identf")
        C.dma("sp", identb[:], d["identb"], w=[identb])
        C.dma("sp", identf[:], d["identf"], w=[identf])
        g1 = C.sb(st, [128, 1024], F32, "g1")
        C.dma("sp", g1[:], d["ln_mix"].partition_broadcast(128), w=[g1])
        if not nsa:
            g2 = C.sb(st, [128, 1024], F32, "g2")
            C.dma("sp", g2[:], d["kv_norm"].partition_broadcast(128), w=[g2])
        wb = C.sb(st, [128, 8, NW], BF16, "wb", nsub=8)
        wst = [C.sb(st, [128, 1304], F32, "wst%d" % i) for i in range(2)]
        for c in range(8):
            w_s = wst[c % 2]
            if nsa:
                C.dma("sp", w_s[:, 0:NW], d["wcat"][c * 128:(c + 1) * 128, :], w=[w_s])
            else:
                C.dma("sp", w_s[:, 0:512], d["wq"][c * 128:(c + 1) * 128, :], w=[w_s])
                C.dma("sp", w_s[:, 512:768], d["wkv"][c * 128:(c + 1) * 128, :], w=[w_s])
            cp(C, "pool", wb[:, c, :], w_s[:, 0:NW], r=[w_s], w=[wb.bs[c]])
        gq = C.sb(st, [128, 512], F32, "gq")
        for h in range(8):
            C.dma("sp", gq[:, h * 64:(h + 1) * 64], d["q_norm"].partition_broadcast(128), w=[gq])
        gk = C.sb(st, [128, 256], F32, "gk")
        if nsa:
            for j, row in enumerate((1, 1, 2, 2)):
                C.dma("sp", gk[:, j * 64:(j + 1) * 64], d["k_norm"][row].partition_broadcast(128), w=[gk])
        else:
            for j in range(2):
                C.dma("sp", gk[:, j * 64:(j + 1) * 64], d["k_norm"].partition_broadcast(128), w=[gk])
            ones256 = C.sb(st, [128, 1], F32, "ones256")
            memset(C, "pool", ones256[:], 1.0 / 256, w=[ones256])
            kmT = C.sb(st, [64, 2, 16], F32, "kmT")
        xt = [C.sb(st, [128, 1024], F32, "xt%d" % i) for i in range(2)]
        junk = C.sb(st, [128, 1024], F32, "junk")
        ss = C.sb(st, [128, 1], F32, "ss")
        xn = C.sb(st, [128, 1024], BF16, "xn")
        xT = C.sb(st, [128, 8, 128], BF16, "xT")
        xn2 = C.sb(st, [128, 1024], BF16, "xn2")
        xT2 = C.sb(st, [128, 8, 128], BF16, "xT2")
        sq = C.sb(st, [128, 512], F32, "sq")
        ssq = C.sb(st, [128, 8], F32, "ssq")
        qn = C.sb(st, [128, 512], BF16, "qn")
        kn = C.sb(st, [128, 256], BF16, "kn")
        knf = C.sb(st, [128, 256], F32, "knf")
        raw = C.sb(st, [128, 256], BF16, "raw")
        qTs = [C.sb(st, [64, 8, 128], BF16, "qTs%d" % i) for i in range(2)]
        kTs = [C.sb(st, [64, 8, 128], BF16, "kTs%d" % i) for i in range(2)]
        va = [C.sb(st, [128, 4, 65], BF16, "va%d" % i) for i in range(2)]
        gts = [C.sb(st, [128, 24], F32, "gts%d" % i) for i in range(2)]
        for i in range(2):
            memset(C, "pool", va[i][:], 1.0, w=[va[i]])
        pT = C.ps(st, [128, 8, 128], BF16, "pT")
        pq = C.ps(st, [128, 512], F32, "pq")
        pk = C.ps(st, [128, 512], F32, "pk")
        pv = C.ps(st, [128, 512], F32, "pv")
        pT2 = C.ps(st, [64, 8, 128], BF16, "pT2")
        pkm = [C.ps(st, [64, 512], F32, "pkm%d" % i) for i in range(2)]

        for t in range(32):
            tk = slice(t * 128, (t + 1) * 128)
            x_t = xt[t % 2]
            C.dma("sp", x_t[:], d["x"][tk, :], w=[x_t])
            act(C, junk[:], x_t[:], AF.Square, r=[x_t], w=[junk])
            C.op("dve", lambda e: e.reduce_sum(out=ss[:], in_=junk[:], axis=AX.X), r=[junk], w=[ss])
            rsqrt_chain(C, ss[:], 1.0 / 1024, r=[ss], w=[ss])
            stt(C, xn[:], x_t[:], ss[:, 0:1], g1[:], ALU.mult, ALU.mult, r=[x_t, ss, g1], w=[xn])
            for c in range(8):
                tr(C, pT[:, c, :], xn[:, c * 128:(c + 1) * 128], identb[:], r=[xn, identb], w=[pT])
            act(C, xT[:], pT[:], AF.Copy, r=[pT], w=[xT])
            if nsa:
                xTk = xT
            else:
                stt(C, xn2[:], x_t[:], ss[:, 0:1], g2[:], ALU.mult, ALU.mult, r=[x_t, ss, g2], w=[xn2])
                for c in range(8):
                    tr(C, pT[:, c, :], xn2[:, c * 128:(c + 1) * 128], identb[:], r=[xn2, identb], w=[pT])
                cp(C, "dve", xT2[:], pT[:], r=[pT], w=[xT2])
                xTk = xT2
            for c in range(8):
                mm(C, pq[:], xT[:, c, :], wb[:, c, 0:512], c == 0, c == 7, r=[xT, wb.bs[c]], w=[pq])
            if nsa:
                for c in range(8):
                    mm(C, pk[:], xT[:, c, :], wb[:, c, 512:1024], c == 0, c == 7, r=[xT, wb.bs[c]], w=[pk])
                for c in range(8):
                    mm(C, pv[:, 0:280], xT[:, c, :], wb[:, c, 1024:1304], c == 0, c == 7, r=[xT, wb.bs[c]], w=[pv])
            else:
                for c in range(8):
                    mm(C, pk[:, 0:256], xTk[:, c, :], wb[:, c, 512:768], c == 0, c == 7, r=[xTk, wb.bs[c]], w=[pk])
            headnorm(C, pq[:], 8, gq, 0.125, sq, ssq, [(qn[:], qn)], pq)
            q_s = qTs[t % 2]
            for h in range(8):
                tr(C, pT2[:, h, :], qn[:, h * 64:(h + 1) * 64], identb[:], r=[qn, identb], w=[pT2])
            act(C, q_s[:], pT2[:], AF.Copy, r=[pT2], w=[q_s])
            C.dma("sp", d["QT"][:, 0:64, tk].rearrange("h d t -> d h t"), q_s[:], r=[q_s])
            k_s = kTs[t % 2]
            v_a = va[t % 2]
            if nsa:
                headnorm(C, pk[:, 0:256], 4, gk, 1.0, sq, ssq, [(kn[:], kn)], pk)
                act(C, raw[:], pk[:, 256:512], AF.Copy, r=[pk], w=[raw])
                for j in range(4):
                    tr(C, pT2[:, j, :], kn[:, j * 64:(j + 1) * 64], identb[:], r=[kn, identb], w=[pT2])
                for j in range(4):
                    tr(C, pT2[:, 4 + j, :], raw[:, j * 64:(j + 1) * 64], identb[:], r=[raw, identb], w=[pT2])
                cp(C, "dve", k_s[:], pT2[:], r=[pT2], w=[k_s])
                C.dma("sp", d["KT"][:, 0:64, tk].rearrange("h d t -> d h t"), k_s[:], r=[k_s])
                act(C, v_a[:, :, 0:64], pv[:, 0:256].rearrange("p (j d) -> p j d", d=64), AF.Copy, r=[pv], w=[v_a])
                C.dma("sp", d["V"][:, tk, :].rearrange("j t d -> t j d"), v_a[:], r=[v_a])
                g_s = gts[t % 2]
                act(C, g_s[:], pv[:, 256:280], AF.Sigmoid, r=[pv], w=[g_s])
                C.dma("sp", d["G"][tk, :], g_s[:], r=[g_s])
            else:
                headnorm(C, pk[:, 0:128], 2, gk, 1.0, sq, ssq, [(kn[:, 0:128], kn), (knf[:, 0:128], knf)], pk)
                for j in range(2):
                    tr(C, pT2[:, j, :], kn[:, j * 64:(j + 1) * 64], identb[:], r=[kn, identb], w=[pT2])
                cp(C, "dve", k_s[:, 0:2, :], pT2[:, 0:2, :], r=[pT2], w=[k_s])
                C.dma("sp", d["KT"][:, 0:64, tk].rearrange("h d t -> d h t"), k_s[:, 0:2, :], r=[k_s])
                act(C, v_a[:, 0:2, 0:64], pk[:, 128:256].rearrange("p (j d) -> p j d", d=64), AF.Copy, r=[pk], w=[v_a])
                C.dma("sp", d["V"][:, tk, :].rearrange("j t d -> t j d"), v_a[:, 0:2, :], r=[v_a])
                for j in range(2):
                    mm(C, pkm[j][:, 0:1], knf[:, j * 64:(j + 1) * 64], ones256[:], t % 2 == 0, t % 2 == 1,
                       r=[knf, ones256], w=[pkm[j]])
                if t % 2 == 1:
                    for j in range(2):
                        cp(C, "dve", kmT[:, j, t // 2:t // 2 + 1], pkm[j][:, 0:1], r=[pkm[j]], w=[kmT])
        if not nsa:
            C.dma("sp", d["KM"].rearrange("g d n -> d g n"), kmT[:], r=[kmT])
    C.P.barrier()


def attn_qk(C, ps_s, QTh, QTb, KTt, KTb, Em, negT, negTb, Emb, bias_ap, biasb, Pt, sel, ncols=512, col0=0):
    cs = slice(col0, col0 + ncols)
    has_m = Em is not None
    mm(C, ps_s[:, cs], KTt, QTh[:, cs], True, not has_m, r=[KTb, QTb], w=[ps_s])
    if has_m:
        mm(C, ps_s[:, cs], Em, negT[:, cs], False, True, r=[Emb, negTb], w=[ps_s])
    act(C, Pt[:, cs], ps_s[:, cs], AF.Exp, r=[ps_s, biasb], w=[Pt], bias=bias_ap)
    if sel is not None:
        pattern, base, cm = sel
        asel(C, Pt[:, cs], Pt[:, cs], pattern, base, cm, r=[Pt], w=[Pt])


def attn_pv(C, Pt, Vt, Vb, ps_o, osubs):
    nv = Vt.shape[-1]
    for (s, st_, sp_) in osubs:
        mm(C, ps_o[s][:, 0:nv], Pt[:, s * 128:(s + 1) * 128], Vt, st_, sp_, r=[Pt, Vb], w=[ps_o[s]])


class Pipe:
    def __init__(self, depth=2):
        self.depth = depth
        self.pend = []

    def push(self, qk, pv):
        qk()
        self.pend.append(pv)
        if len(self.pend) > self.depth:
            self.pend.pop(0)()

    def flush(self):
        for f in self.pend:
            f()
        self.pend = []


def attn_tile(C, pipe, ps_s, QTh, QTb, KTt, KTb, Em, negT, negTb, Emb, bias_ap, biasb, Pt, sel, Vt, Vb, ps_o, osubs,
              ncols=512, col0=0):
    pipe.push(lambda: attn_qk(C, ps_s, QTh, QTb, KTt, KTb, Em, negT, negTb, Emb, bias_ap, biasb, Pt, sel, ncols, col0),
              lambda: attn_pv(C, Pt, Vt, Vb, ps_o, osubs))


def attn_moba(C, d):
    with ExitStack() as st:
        identb = C.sb(st, [128, 128], BF16, "identb")
        C.dma("sp", identb[:], d["identb"], w=[identb])
        biask = C.sb(st, [128, 8, 36], F32, "biask")
        C.dma("sp", biask[:], d["biask"], w=[biask])
        addM = C.sb(st, [128, 32, 16], F32, "addM")
        C.dma("sp", addM[:], d["addM"].rearrange("t p n -> p t n"), w=[addM])
        own01 = C.sb(st, [128, 32, 16], F32, "own01")
        C.dma("sp", own01[:], d["own01"].rearrange("t p n -> p t n"), w=[own01])
        kmf = C.sb(st, [64, 2, 16], F32, "kmf")
        C.dma("sp", kmf[:], d["KM"].rearrange("g d n -> d g n"), w=[kmf])
        kmb = C.sb(st, [64, 2, 16], BF16, "kmb")
        cp(C, "dve", kmb[:], kmf[:], r=[kmf], w=[kmb])
        KT = C.sb(st, [80, 4096], BF16, "KT")
        C.dma("sp", KT[64:80, :], d["Emoba"].rearrange("n t m -> n (t m)"), w=[KT])
        V = C.sb(st, [128, 32, 65], BF16, "V")
        QT = [C.sb(st, [80, 4, 512], BF16, "QT%d" % i, nsub=4) for i in range(2)]
        qshB = C.sb(st, [80, 4, 512], BF16, "qshB")
        Pt = [C.sb(st, [128, 512], BF16, "Pt%d" % i) for i in range(4)]
        sgm = C.sb(st, [128, 4, 16], F32, "sgm")
        m8 = C.sb(st, [128, 4, 8], F32, "m8")
        thr = C.sb(st, [128, 4], F32, "thr")
        negm = [C.sb(st, [128, 4, 80], BF16, "negm%d" % i) for i in range(2)]
        for i in range(2):
            memset(C, "pool", negm[i][:], 0.0, w=[negm[i]])
        rden = C.sb(st, [128, 4], F32, "rden")
        acc = [C.sb(st, [128, 4, 256], BF16, "acc%d" % i) for i in range(2)]
        ps_s = [C.ps(st, [128, 512], F32, "ps_s%d" % i) for i in range(2)]
        ps_o = [C.ps(st, [128, 512], F32, "ps_o%d" % i) for i in range(4)]
        ps_g = C.ps(st, [128, 4, 16], F32, "ps_g")
        ps_t = C.ps(st, [80, 512], BF16, "ps_t")
        npt = 0
        nq = 0
        nh = 0
        pipe = Pipe(2)
        for g in range(2):
            C.dma("sp", KT[0:64, :], d["KT"][g, 0:64, :], w=[KT])
            C.dma("sp", V[:], d["V"][g].rearrange("(kt p) e -> p kt e", p=128), w=[V])
            for hh in range(4):
                C.dma("sp", qshB[64:80, hh, :], d["qshift"][4 * g + hh].partition_broadcast(16), w=[qshB])
            for qc in range(8):
                Q0 = qc * 512
                Q = QT[nq % 2]
                a_c = acc[nq % 2]
                nq += 1
                C.dma("sp", Q[0:64, :, :], d["QT"][4 * g:4 * g + 4, 0:64, Q0:Q0 + 512].rearrange("h d t -> d h t"),
                      w=Q.bs)
                for hh in range(4):
                    h = 4 * g + hh
                    n_m = negm[nh % 2]
                    nh += 1
                    Qb = Q.bs[hh]
                    for s in range(4):
                        mm(C, ps_g[:, s, :], Q[0:64, hh, s * 128:(s + 1) * 128], kmb[:, g, :], True, True,
                           r=[Qb, kmb], w=[ps_g])
                    tt(C, "dve", sgm[:], ps_g[:], addM[:, 4 * qc:4 * qc + 4, :], ALU.add, r=[ps_g, addM], w=[sgm])
                    for s in range(4):
                        C.op("dve", lambda e, s=s: e.max(out=m8[:, s, :], in_=sgm[:, s, :]), r=[sgm], w=[m8])
                    ts(C, "dve", thr[:], m8[:, :, 2], -1e29, None, ALU.max, None, r=[m8], w=[thr])
                    tt(C, "dve", sgm[:], sgm[:], thr[:].unsqueeze(2).to_broadcast([128, 4, 16]), ALU.is_ge,
                       r=[sgm, thr], w=[sgm])
                    tt(C, "dve", sgm[:], sgm[:], own01[:, 4 * qc:4 * qc + 4, :], ALU.max, r=[sgm, own01], w=[sgm])
                    ts(C, "dve", n_m[:, :, 64:80], sgm[:], -1.0, -NEG, ALU.add, ALU.mult, r=[sgm], w=[n_m])
                    for s in range(4):
                        tr(C, ps_t[:, s * 128:(s + 1) * 128], n_m[:, s, :], identb[:], r=[n_m, identb], w=[ps_t])
                    tt(C, "dve", Q[64:80, hh, :], ps_t[64:80, :], qshB[64:80, hh, :], ALU.add, r=[ps_t, qshB], w=[Qb])
                    for kt in range(4 * qc + 4):
                        P_ = Pt[npt % 4]
                        p_s = ps_s[npt % 2]
                        npt += 1
                        sel = ([[1, 512]], Q0 - 128 * kt, -1) if kt >= 4 * qc else None
                        osubs = [(s, kt == 0, kt == 4 * qc + s) for s in range(4) if kt <= 4 * qc + s]
                        attn_tile(C, pipe, p_s, Q[:, hh, :], Qb, KT[:, kt * 128:(kt + 1) * 128], KT, None, None,
                                  None, None, biask[:, h, kt - 4 * qc + 32:kt - 4 * qc + 33], biask, P_, sel,
                                  V[:, kt, :], V, ps_o, osubs)
                    pipe.flush()
                    for s in range(4):
                        C.op("dve", lambda e, s=s: e.reciprocal(out=rden[:, s:s + 1], in_=ps_o[s][:, 64:65]),
                             r=[ps_o[s]], w=[rden])
                        ts(C, "dve", a_c[:, s, hh * 64:(hh + 1) * 64], ps_o[s][:, 0:64], rden[:, s:s + 1], None,
                           ALU.mult, None, r=[ps_o[s], rden], w=[a_c])
                C.dma("sp", d["o"][Q0:Q0 + 512, g * 256:(g + 1) * 256].rearrange("(s p) c -> p s c", p=128),
                      a_c[:], r=[a_c])
    C.P.barrier()


def compress_stage(C, d, kcT, vcaug, identb):
    with ExitStack() as st:
        rawT = [C.sb(st, [64, 4096], BF16, "rawT%d" % i) for i in range(2)]
        wst = [C.sb(st, [64, 8, 256], F32, "cwst%d" % i) for i in range(2)]
        W1b = C.sb(st, [64, 32, 256], BF16, "W1b", nsub=4)
        w2f = C.sb(st, [128, 2, 64], F32, "w2f")
        W2b = C.sb(st, [128, 2, 64], BF16, "W2b")
        posf = C.sb(st, [64, 32], F32, "posf")
        posT = C.sb(st, [64, 32], BF16, "posT")
        c1s = C.sb(st, [128, 2], F32, "c1s")
        u = C.sb(st, [128, 256], F32, "u")
        u2 = C.sb(st, [128, 256], F32, "u2")
        sgt = C.sb(st, [128, 256], F32, "sgt")
        g1T = C.sb(st, [128, 2, 256], BF16, "g1T")
        gkc = C.sb(st, [128, 64], F32, "gkc")
        sq = C.sb(st, [128, 64], F32, "csq")
        ssq = C.sb(st, [128, 8], F32, "cssq")
        knc = C.sb(st, [128, 64], BF16, "knc")
        ovl = C.sb(st, [128, 2, 64], BF16, "ovl")
        ps_c1 = C.ps(st, [128, 2], F32, "ps_c1")
        ps_h = [C.ps(st, [128, 512], F32, "ps_h%d" % i) for i in range(2)]
        ps_k = C.ps(st, [128, 64], F32, "ps_k")
        ps_tk = C.ps(st, [64, 128], BF16, "ps_tk")
        C.dma("sp", gkc[:], d["k_norm"][0].partition_broadcast(128), w=[gkc])
        C.dma("sp", ovl[:], d["overlap"].rearrange("(t p) j -> p t j", p=128), w=[ovl])
        memset(C, "pool", g1T[:], 0.0, w=[g1T])
        nr = 0
        for kind in range(2):
            pre = "ck" if kind == 0 else "cv"
            w1v = d[pre + "_w1"].rearrange("(l dd) h -> dd l h", dd=64)
            for i in range(4):
                w_s = wst[i % 2]
                C.dma("sp", w_s[:], w1v[:, 8 * i:8 * i + 8, :], w=[w_s])
                cp(C, "pool", W1b[:, 8 * i:8 * i + 8, :], w_s[:], r=[w_s], w=[W1b.bs[i]])
            C.dma("sp", w2f[:], d[pre + "_w2"].rearrange("(c p) dd -> p c dd", p=128), w=[w2f])
            cp(C, "dve", W2b[:], w2f[:], r=[w2f], w=[W2b])
            C.dma("sp", posf[:], d[pre + "_pos"].rearrange("l dd -> dd l"), w=[posf], allow_slow_non_contiguous=True)
            cp(C, "dve", posT[:], posf[:], r=[posf], w=[posT])
            for hc in range(2):
                for l in range(32):
                    mm(C, ps_c1[:, hc:hc + 1], W1b[:, l, hc * 128:(hc + 1) * 128], posT[:, l:l + 1], l == 0, l == 31,
                       r=[W1b.bs[l // 8], posT], w=[ps_c1])
            cp(C, "dve", c1s[:], ps_c1[:], r=[ps_c1], w=[c1s])
            for g in range(2):
                r_T = rawT[nr % 2]
                nr += 1
                C.dma("sp", r_T[:], d["KT"][4 + 2 * kind + g, 0:64, :], w=[r_T])
                r3 = r_T[:].rearrange("p (n s) -> p n s", s=16)
                for hc in range(2):
                    for l in range(32):
                        rhs = r3[:, 0:255, l] if l < 16 else r3[:, 1:256, l - 16]
                        mm(C, ps_h[hc][:, 0:255], W1b[:, l, hc * 128:(hc + 1) * 128], rhs, l == 0, l == 31,
                           r=[W1b.bs[l // 8], r_T], w=[ps_h[hc]])
                    ts(C, "dve", u[:, 0:255], ps_h[hc][:, 0:255], c1s[:, hc:hc + 1], None, ALU.add, None,
                       r=[ps_h[hc], c1s], w=[u])
                    tt(C, "pool", u2[:, 0:255], u[:, 0:255], u[:, 0:255], ALU.mult, r=[u], w=[u2])
                    ts(C, "pool", u2[:, 0:255], u2[:, 0:255], 0.044715, 1.0, ALU.mult, ALU.add, r=[u2], w=[u2])
                    tt(C, "pool", u2[:, 0:255], u2[:, 0:255], u[:, 0:255], ALU.mult, r=[u2, u], w=[u2])
                    act(C, sgt[:, 0:255], u2[:, 0:255], AF.Sigmoid, r=[u2], w=[sgt], scale=1.5957691216057308)
                    tt(C, "dve", g1T[:, hc, 0:255], u[:, 0:255], sgt[:, 0:255], ALU.mult, r=[u, sgt], w=[g1T])
                for nt in range(2):
                    for hc in range(2):
                        mm(C, ps_k[:], g1T[:, hc, nt * 128:(nt + 1) * 128], W2b[:, hc, :], hc == 0, hc == 1,
                           r=[g1T, W2b], w=[ps_k])
                    if kind == 0:
                        headnorm(C, ps_k[:], 1, gkc, 1.0, sq, ssq, [(knc[:], knc)], ps_k)
                        tr(C, ps_tk[:], knc[:], identb[:], r=[knc, identb], w=[ps_tk])
                        act(C, kcT[g][0:64, nt * 128:(nt + 1) * 128], ps_tk[:], AF.Copy, r=[ps_tk], w=[kcT[g]])
                    else:
                        act(C, vcaug[g][:, nt, 0:64], ps_k[:], AF.Copy, r=[ps_k], w=[vcaug[g]])
        for g in range(2):
            memset(C, "pool", kcT[g][64:65, :], 1.0, w=[kcT[g]])
            memset(C, "pool", vcaug[g][:, :, 128:129], 1.0, w=[vcaug[g]])
            cp(C, "pool", vcaug[g][:, :, 64:128], ovl[:], r=[ovl], w=[vcaug[g]])
    C.P.barrier()


def attn_nsa(C, d):
    with ExitStack() as st0:
        identb = C.sb(st0, [128, 128], BF16, "identb")
        C.dma("sp", identb[:], d["identb"], w=[identb])
        kcT = [C.sb(st0, [65, 256], BF16, "kcT%d" % i) for i in range(2)]
        vcaug = [C.sb(st0, [128, 2, 129], BF16, "vcaug%d" % i) for i in range(2)]
        compress_stage(C, d, kcT, vcaug, identb)
        with ExitStack() as st:
            biask = C.sb(st, [128, 8, 36], F32, "biask")
            C.dma("sp", biask[:], d["biask"], w=[biask])
            biasc = C.sb(st, [128, 8, 8, 2], F32, "biasc")
            C.dma("sp", biasc[:], d["biasc"], w=[biasc])
            KsT = C.sb(st, [128, 4096], BF16, "KsT")
            C.dma("sp", KsT[64:128, :], d["Esel"].rearrange("n t m -> n (t m)"), w=[KsT])
            Qs = [C.sb(st, [128, 4, 512], BF16, "Qs%d" % i, nsub=4) for i in range(2)]
            qshB = C.sb(st, [128, 4, 512], BF16, "qshB")
            KwT = C.sb(st, [65, 4096], BF16, "KwT")
            Vs = C.sb(st, [128, 32, 65], BF16, "Vs")
            Vw = C.sb(st, [128, 32, 65], BF16, "Vw")
            QT = [C.sb(st, [65, 4, 512], BF16, "QT%d" % i) for i in range(2)]
            gt = [C.sb(st, [128, 4, 12], F32, "gt%d" % i) for i in range(2)]
            addT = [C.sb(st, [128, 4, 64], F32, "addT%d" % i) for i in range(2)]
            Pt = [C.sb(st, [128, 512], BF16, "Pt%d" % i) for i in range(4)]
            acc = C.sb(st, [128, 4, 256], F32, "acc")
            accb = [C.sb(st, [128, 4, 256], BF16, "accb%d" % i) for i in range(2)]
            imp = C.sb(st, [128, 4, 64], F32, "imp")
            sc = C.sb(st, [128, 64], F32, "sc")
            sc2 = C.sb(st, [128, 64], F32, "sc2")
            m8 = C.sb(st, [128, 16], F32, "m8")
            negm = C.sb(st, [128, 128], BF16, "negm")
            memset(C, "pool", negm[:], 0.0, w=[negm])
            sm = C.sb(st, [128, 16], F32, "sm")
            ps_s = [C.ps(st, [128, 512], F32, "ps_s%d" % i) for i in range(2)]
            ps_o = [C.ps(st, [128, 512], F32, "ps_o%d" % i) for i in range(4)]
            ps_t = C.ps(st, [128, 512], BF16, "ps_t")
            npt = 0
            nq = 0
            pipe = Pipe(2)
            for g in range(2):
                C.dma("sp", KsT[0:64, :], d["KT"][g, 0:64, :], w=[KsT])
                C.dma("sp", KwT[0:64, :], d["KT"][2 + g, 0:64, :], w=[KwT])
                for hh in range(4):
                    C.dma("sp", qshB[64:128, hh, :], d["qshift"][4 * g + hh].partition_broadcast(64), w=[qshB])
                memset(C, "pool", KwT[64:65, :], 1.0, w=[KwT])
                C.dma("sp", Vs[:], d["V"][g].rearrange("(kt p) e -> p kt e", p=128), w=[Vs])
                C.dma("sp", Vw[:], d["V"][2 + g].rearrange("(kt p) e -> p kt e", p=128), w=[Vw])
                for qc in range(8):
                    Q0 = qc * 512
                    Q = QT[nq % 2]
                    Q_s = Qs[nq % 2]
                    g_t = gt[nq % 2]
                    a_T = addT[nq % 2]
                    a_b = accb[nq % 2]
                    nq += 1
                    C.dma("sp", Q[0:64, :, :],
                          d["QT"][4 * g:4 * g + 4, 0:64, Q0:Q0 + 512].rearrange("h dd t -> dd h t"), w=[Q])
                    C.dma("sp", Q[64:65, :, :], d["qshift"][4 * g:4 * g + 4, :].unsqueeze(0), w=[Q])
                    C.dma("sp", Q_s[0:64, :, :],
                          d["QT"][4 * g:4 * g + 4, 0:64, Q0:Q0 + 512].rearrange("h dd t -> dd h t"), w=Q_s.bs)
                    C.dma("sp", g_t[:], d["G"][Q0:Q0 + 512, 12 * g:12 * g + 12].rearrange("(s p) c -> p s c", p=128),
                          w=[g_t])
                    C.dma("sp", a_T[:], d["addT"][4 * qc:4 * qc + 4].rearrange("t p n -> p t n"), w=[a_T])
                    nts = 2 if qc >= 4 else 1
                    for hh in range(4):
                        h = 4 * g + hh
                        for nt in range(nts):
                            P_ = Pt[npt % 4]
                            p_s = ps_s[npt % 2]
                            npt += 1
                            osubs = [(s, nt == 0, nt == nts - 1) for s in range(4)]
                            attn_tile(C, pipe, p_s, Q[:, hh, :], Q, kcT[g][:, nt * 128:(nt + 1) * 128], kcT[g], None, None,
                                      None, None, biasc[:, h, qc, nt:nt + 1], biasc, P_,
                                      ([[1, 512]], Q0 - 31 - 2048 * nt, -16), vcaug[g][:, nt, :], vcaug[g], ps_o,
                                      osubs)
                        pipe.flush()
                        for s in range(4):
                            ts(C, "dve", sm[:, 0:1], ps_o[s][:, 128:129], 1e-30, None, ALU.max, None,
                               r=[ps_o[s]], w=[sm])
                            C.op("dve", lambda e: e.reciprocal(out=sm[:, 1:2], in_=sm[:, 0:1]), r=[sm], w=[sm])
                            tt(C, "dve", sm[:, 2:3], sm[:, 1:2], g_t[:, s, 3 * hh:3 * hh + 1], ALU.mult,
                               r=[sm, g_t], w=[sm])
                            ts(C, "dve", acc[:, s, hh * 64:(hh + 1) * 64], ps_o[s][:, 0:64], sm[:, 2:3], None,
                               ALU.mult, None, r=[ps_o[s], sm], w=[acc])
                            if hh == 0:
                                ts(C, "dve", imp[:, s, :], ps_o[s][:, 64:128], sm[:, 1:2], None, ALU.mult, None,
                                   r=[ps_o[s], sm], w=[imp])
                            else:
                                stt(C, imp[:, s, :], ps_o[s][:, 64:128], sm[:, 1:2], imp[:, s, :], ALU.mult, ALU.add,
                                    r=[ps_o[s], sm, imp], w=[imp])
                    for s in range(4):
                        tt(C, "dve", sc[:], imp[:, s, :], a_T[:, s, :], ALU.add, r=[imp, a_T], w=[sc])
                        C.op("dve", lambda e: e.max(out=m8[:, 0:8], in_=sc[:]), r=[sc], w=[m8])
                        C.op("dve", lambda e: e.match_replace(out=sc2[:], in_to_replace=m8[:, 0:8], in_values=sc[:],
                                                              imm_value=-3.0e38), r=[sc, m8], w=[sc2])
                        C.op("dve", lambda e: e.max(out=m8[:, 8:16], in_=sc2[:]), r=[sc2], w=[m8])
                        ts(C, "dve", sm[:, 4:5], m8[:, 15:16], -1e29, None, ALU.max, None, r=[m8], w=[sm])
                        ts(C, "dve", sc2[:], sc[:], sm[:, 4:5], None, ALU.is_ge, None, r=[sc, sm], w=[sc2])
                        ts(C, "dve", negm[:, 64:128], sc2[:], -1.0, -NEG, ALU.add, ALU.mult, r=[sc2], w=[negm])
                        tr(C, ps_t[:, s * 128:(s + 1) * 128], negm[:], identb[:], r=[negm, identb], w=[ps_t])
                    for hh in range(4):
                        tt(C, "dve", Q_s[64:128, hh, :], ps_t[64:128, :], qshB[64:128, hh, :], ALU.add,
                           r=[ps_t, qshB], w=[Q_s.bs[hh]])
                    for hh in range(4):
                        h = 4 * g + hh
                        for kt in range(4 * qc + 4):
                            P_ = Pt[npt % 4]
                            p_s = ps_s[npt % 2]
                            npt += 1
                            sel = ([[1, 512]], Q0 - 128 * kt, -1) if kt >= 4 * qc else None
                            osubs = [(s, kt == 0, kt == 4 * qc + s) for s in range(4) if kt <= 4 * qc + s]
                            attn_tile(C, pipe, p_s, Q_s[:, hh, :], Q_s.bs[hh], KsT[:, kt * 128:(kt + 1) * 128], KsT,
                                      None, None, None, None, biask[:, h, kt - 4 * qc + 32:kt - 4 * qc + 33], biask,
                                      P_, sel, Vs[:, kt, :], Vs, ps_o, osubs)
                        pipe.flush()
                        for s in range(4):
                            C.op("dve", lambda e, s=s: e.reciprocal(out=sm[:, 1:2], in_=ps_o[s][:, 64:65]),
                                 r=[ps_o[s]], w=[sm])
                            tt(C, "dve", sm[:, 2:3], sm[:, 1:2], g_t[:, s, 3 * hh + 1:3 * hh + 2], ALU.mult,
                               r=[sm, g_t], w=[sm])
                            stt(C, acc[:, s, hh * 64:(hh + 1) * 64], ps_o[s][:, 0:64], sm[:, 2:3],
                                acc[:, s, hh * 64:(hh + 1) * 64], ALU.mult, ALU.add, r=[ps_o[s], sm, acc], w=[acc])
                    for hh in range(4):
                        h = 4 * g + hh
                        for kt in range(max(0, 4 * qc - 4), 4 * qc + 4):
                            rel = kt - 4 * qc
                            s_lo, s_hi = max(0, rel), min(3, rel + 4)
                            col0, ncols = 128 * s_lo, 128 * (s_hi - s_lo + 1)
                            P_ = Pt[npt % 4]
                            p_s = ps_s[npt % 2]
                            npt += 1
                            if rel < 0:
                                sel = ([[-1, ncols]], 128 * kt - Q0 + 511 - col0, 1)
                            else:
                                sel = ([[1, ncols]], Q0 - 128 * kt + col0, -1)
                            osubs = [(s, kt == max(0, 4 * qc + s - 4), kt == 4 * qc + s) for s in range(s_lo, s_hi + 1)]
                            attn_tile(C, pipe, p_s, Q[:, hh, :], Q, KwT[:, kt * 128:(kt + 1) * 128], KwT, None, None, None,
                                      None, biask[:, h, rel + 32:rel + 33], biask, P_, sel, Vw[:, kt, :], Vw, ps_o,
                                      osubs, ncols=ncols, col0=col0)
                        pipe.flush()
                        for s in range(4):
                            C.op("dve", lambda e, s=s: e.reciprocal(out=sm[:, 1:2], in_=ps_o[s][:, 64:65]),
                                 r=[ps_o[s]], w=[sm])
                            tt(C, "dve", sm[:, 2:3], sm[:, 1:2], g_t[:, s, 3 * hh + 2:3 * hh + 3], ALU.mult,
                               r=[sm, g_t], w=[sm])
                            stt(C, acc[:, s, hh * 64:(hh + 1) * 64], ps_o[s][:, 0:64], sm[:, 2:3],
                                acc[:, s, hh * 64:(hh + 1) * 64], ALU.mult, ALU.add, r=[ps_o[s], sm, acc], w=[acc])
                    act(C, a_b[:], acc[:], AF.Copy, r=[acc], w=[a_b])
                    C.dma("sp", d["o"][Q0:Q0 + 512, g * 256:(g + 1) * 256].rearrange("(s p) c -> p s c", p=128),
                          a_b[:], r=[a_b])
    C.P.barrier()


import ml_dtypes
from concourse.bass_utils import run_bass_kernel_spmd

_bf = ml_dtypes.bfloat16


def _consts_common(hp):
    slopes = 2.0 ** (-(np.arange(16) + 1) / 2.0)
    sl = slopes[8 * hp:8 * hp + 8]
    qshift = (-sl[:, None] * np.arange(512)[None, :]).astype(np.float32).astype(_bf)
    j = np.arange(36) - 32
    biask = (sl[None, :, None] * (128.0 * j[None, None, :] + np.arange(128)[:, None, None])).astype(np.float32)
    return dict(qshift=qshift, biask=biask, identb=np.eye(128, dtype=np.float32).astype(_bf),
                identf=np.eye(128, dtype=np.float32))


def _consts_moba():
    E = np.zeros((16, 32, 128), np.float32)
    for kt in range(32):
        E[kt // 2, kt, :] = 1
    addM = np.zeros((32, 128, 16), np.float32)
    own = np.zeros((32, 128, 16), np.float32)
    for qt in range(32):
        cb = qt // 2
        addM[qt, :, cb:] = -1e30
        own[qt, :, cb] = 1
    return dict(Emoba=E.astype(_bf), addM=addM, own01=own)


def _consts_nsa(hp):
    slopes = 2.0 ** (-(np.arange(16) + 1) / 2.0)
    sl = slopes[8 * hp:8 * hp + 8]
    E = np.zeros((64, 32, 128), np.float32)
    for kt in range(32):
        E[2 * kt, kt, 0:64] = 1
        E[2 * kt + 1, kt, 64:128] = 1
    p = np.arange(128)
    biasc = np.zeros((128, 8, 8, 2), np.float32)
    for qc in range(8):
        for nt in range(2):
            biasc[:, :, qc, nt] = sl[None, :] * (16.0 * (p[:, None] + 128 * nt) + 31 - 512 * qc)
    addT = np.zeros((32, 128, 64), np.float32)
    for qt in range(32):
        for half in range(2):
            cur = 2 * qt + half
            rows = slice(64 * half, 64 * half + 64)
            addT[qt, rows, cur + 1:] = -1e30
            for j in (0, cur, cur - 1):
                if j >= 0:
                    addT[qt, rows, j] = 1e9
    n = np.arange(256)[:, None]
    j = np.arange(64)[None, :]
    ov = np.clip(np.minimum(16 * n + 32, 64 * j + 64) - np.maximum(16 * n, 64 * j), 0, None) / 32.0
    ov[255] = 0
    return dict(Esel=E.astype(_bf), biasc=biasc, addT=addT, overlap=ov.astype(np.float32).astype(_bf))


def _din(nc, name, shape, dt=F32):
    return nc.dram_tensor(name, list(shape), dt, kind="ExternalInput").ap()


def _dint(nc, name, shape, dt):
    return nc.dram_tensor(name, list(shape), dt, kind="Internal").ap()


def _decl_A0(nc):
    return dict(x=_din(nc, "x", [4096, 1024]), wcat=_din(nc, "wcat", [1024, 1304]), ln_mix=_din(nc, "ln_mix", [1024]),
                q_norm=_din(nc, "q_norm", [64]), k_norm=_din(nc, "k_norm", [3, 64]),
                ck_pos=_din(nc, "ck_pos", [32, 64]), ck_w1=_din(nc, "ck_w1", [2048, 256]),
                ck_w2=_din(nc, "ck_w2", [256, 64]), cv_pos=_din(nc, "cv_pos", [32, 64]),
                cv_w1=_din(nc, "cv_w1", [2048, 256]), cv_w2=_din(nc, "cv_w2", [256, 64]),
                identb=_din(nc, "identb", [128, 128], BF16), identf=_din(nc, "identf", [128, 128]),
                qshift=_din(nc, "qshift", [8, 512], BF16), biask=_din(nc, "biask", [128, 8, 36]),
                Esel=_din(nc, "Esel", [64, 32, 128], BF16), biasc=_din(nc, "biasc", [128, 8, 8, 2]),
                addT=_din(nc, "addT", [32, 128, 64]), overlap=_din(nc, "overlap", [256, 64], BF16),
                QT=_dint(nc, "QT", [8, 64, 4096], BF16), KT=_dint(nc, "KT", [8, 64, 4096], BF16),
                V=_dint(nc, "V", [4, 4096, 65], BF16), G=_dint(nc, "G", [4096, 24], F32))


def _decl_A1(nc):
    return dict(x=_din(nc, "x", [4096, 1024]), wq=_din(nc, "wq", [1024, 512]), wkv=_din(nc, "wkv", [1024, 256]),
                ln_mix=_din(nc, "ln_mix", [1024]), kv_norm=_din(nc, "kv_norm", [1024]),
                q_norm=_din(nc, "q_norm", [64]), k_norm=_din(nc, "k_norm", [64]),
                identb=_din(nc, "identb", [128, 128], BF16), identf=_din(nc, "identf", [128, 128]),
                qshift=_din(nc, "qshift", [8, 512], BF16), biask=_din(nc, "biask", [128, 8, 36]),
                Emoba=_din(nc, "Emoba", [16, 32, 128], BF16), addM=_din(nc, "addM", [32, 128, 16]),
                own01=_din(nc, "own01", [32, 128, 16]),
                QT=_dint(nc, "QT", [8, 64, 4096], BF16), KT=_dint(nc, "KT", [2, 64, 4096], BF16),
                V=_dint(nc, "V", [2, 4096, 65], BF16), KM=_dint(nc, "KM", [2, 64, 16], F32))


def _decl_B(nc, NT):
    return dict(hin=_din(nc, "hin", [NT, 1024]), o=_din(nc, "o", [NT, 1024], BF16), p=_din(nc, "p", [NT, 256]),
                w_out=_din(nc, "w_out", [1024, 1024]), ln_ffn=_din(nc, "ln_ffn", [1024]),
                ln_ple=_din(nc, "ln_ple", [1024]), w_group=_din(nc, "w_group", [1024, 4]),
                b_group=_din(nc, "b_group", [4]), w_expert=_din(nc, "w_expert", [1024, 32]),
                b_expert=_din(nc, "b_expert", [32]), w_gate=_din(nc, "w_gate", [32, 1024, 128]),
                w_up=_din(nc, "w_up", [32, 1024, 128]), w_down=_din(nc, "w_down", [32, 128, 1024]),
                ple_w_proj=_din(nc, "ple_w_proj", [256, 1024]), ple_w_gate=_din(nc, "ple_w_gate", [1024, 1024]),
                identb=_din(nc, "identb", [128, 128], BF16), identf=_din(nc, "identf", [128, 128]),
                sele=_din(nc, "sele", [32, 32, 128], BF16))


def _build(which):
    nc = bass.Bass("TRN2", target_bir_lowering=False)
    with ExitStack() as st:
        P = Prog(nc, st)
        C = Ctx(nc, P)
        if which == "A0":
            d = _decl_A0(nc)
            d["o"] = nc.dram_tensor("o", [4096, 512], BF16, kind="ExternalOutput").ap()
            prep(C, d, "nsa")
            attn_nsa(C, d)
        elif which == "A1":
            d = _decl_A1(nc)
            d["o"] = nc.dram_tensor("o", [4096, 512], BF16, kind="ExternalOutput").ap()
            prep(C, d, "moba")
            attn_moba(C, d)
        else:
            d = _decl_B(nc, 2048)
            d["hout"] = nc.dram_tensor("hout", [2048, 1024], F32, kind="ExternalOutput").ap()
            phase_B(C, 2048, d)
        P.emit()
    return nc


def _wcat_for(w, hp):
    q = w[:, 512 * hp:512 * hp + 512]

    def kv(i):
        base = 1024 + 256 * i
        return w[:, base + 128 * hp: base + 128 * hp + 128]
    kc, vc, ks, vs, kw, vw = [kv(i) for i in range(6)]
    gl = w[:, 2560 + 24 * hp: 2560 + 24 * hp + 24]
    return np.ascontiguousarray(np.concatenate([q, ks, kw, kc, vc, vs, vw, gl], 1))


def _run_B(I, L, h, o, w_out):
    sele = np.zeros((32, 32, 128), np.float32)
    for e in range(32):
        sele[e, e, :] = 1
    consts = dict(identb=np.eye(128, dtype=np.float32).astype(_bf), identf=np.eye(128, dtype=np.float32),
                  sele=sele.astype(_bf))
    maps = []
    for c in range(8):
        b, hf = c // 2, c % 2
        tk = slice(hf * 2048, hf * 2048 + 2048)
        maps.append(dict(hin=np.ascontiguousarray(h[b, tk]), o=np.ascontiguousarray(o[b, tk]),
                         p=np.ascontiguousarray(I['p'][L, b, tk]), w_out=w_out, ln_ffn=I['ln_ffn'][L],
                         ln_ple=I['ln_ple'][L], w_group=I['moe_w_group'][L], b_group=I['moe_b_group'][L],
                         w_expert=I['moe_w_expert'][L], b_expert=I['moe_b_expert'][L], w_gate=I['moe_w_gate'][L],
                         w_up=I['moe_w_up'][L], w_down=I['moe_w_down'][L], ple_w_proj=I['ple_w_proj'][L],
                         ple_w_gate=I['ple_w_gate'][L], **consts))
    res = run_bass_kernel_spmd(_build("B"), maps, core_ids=list(range(8)))
    out = np.empty((4, 4096, 1024), np.float32)
    for c in range(8):
        b, hf = c // 2, c % 2
        out[b, hf * 2048:hf * 2048 + 2048] = res.results[c]["hout"]
    return out


def _gather_o(res):
    o = np.empty((4, 4096, 1024), _bf)
    for c in range(8):
        b, hp = c // 2, c % 2
        o[b, :, 512 * hp:512 * hp + 512] = res.results[c]["o"]
    return o


def kernel_unfused(**inputs):
    I = {k: np.asarray(v) for k, v in inputs.items()}
    x = I['x']
    maps = []
    for c in range(8):
        b, hp = c // 2, c % 2
        maps.append(dict(x=np.ascontiguousarray(x[b]), wcat=_wcat_for(I['a_w_in'][0], hp), ln_mix=I['ln_mix'][0],
                         q_norm=I['a_q_norm'][0], k_norm=I['a_k_norm'][0], ck_pos=I['a_ck_pos'][0],
                         ck_w1=I['a_ck_w1'][0], ck_w2=I['a_ck_w2'][0], cv_pos=I['a_cv_pos'][0],
                         cv_w1=I['a_cv_w1'][0], cv_w2=I['a_cv_w2'][0], **_consts_common(hp), **_consts_nsa(hp)))
    o0 = _gather_o(run_bass_kernel_spmd(_build("A0"), maps, core_ids=list(range(8))))
    h0 = _run_B(I, 0, x, o0, I['a_w_out'][0])
    maps = []
    cm = _consts_moba()
    wkv = I['w_kv_shared']
    for c in range(8):
        b, hp = c // 2, c % 2
        maps.append(dict(x=np.ascontiguousarray(h0[b]),
                         wq=np.ascontiguousarray(I['b_w_q'][0][:, 512 * hp:512 * hp + 512]),
                         wkv=np.ascontiguousarray(np.concatenate(
                             [wkv[:, 128 * hp:128 * hp + 128], wkv[:, 256 + 128 * hp:256 + 128 * hp + 128]], 1)),
                         ln_mix=I['ln_mix'][1], kv_norm=I['kv_norm'], q_norm=I['b_q_norm'][0],
                         k_norm=I['k_norm_shared'], **_consts_common(hp), **cm))
    o1 = _gather_o(run_bass_kernel_spmd(_build("A1"), maps, core_ids=list(range(8))))
    return _run_B(I, 1, h0, o1, I['b_w_out'][0])


def _build_fused():
    nc = bass.Bass("TRN2", target_bir_lowering=False)
    d0 = dict(x=_din(nc, "x", [4096, 1024]), q_norm=_din(nc, "q_norm", [64]), k_norm=_din(nc, "k_norm", [3, 64]),
              ck_pos=_din(nc, "ck_pos", [32, 64]), ck_w1=_din(nc, "ck_w1", [2048, 256]),
              ck_w2=_din(nc, "ck_w2", [256, 64]), cv_pos=_din(nc, "cv_pos", [32, 64]),
              cv_w1=_din(nc, "cv_w1", [2048, 256]), cv_w2=_din(nc, "cv_w2", [256, 64]),
              identb=_din(nc, "identb", [128, 128], BF16), identf=_din(nc, "identf", [128, 128]),
              QT=_dint(nc, "QT", [8, 64, 4096], BF16), KT=_dint(nc, "KT", [8, 64, 4096], BF16),
              V=_dint(nc, "V", [4, 4096, 65], BF16), G=_dint(nc, "G", [4096, 24], F32))
    per_hp = []
    for hp in range(2):
        per_hp.append(dict(
            wcat=_din(nc, "wcat%d" % hp, [1024, 1304]), qshift=_din(nc, "qshift%d" % hp, [8, 512], BF16),
            biask=_din(nc, "biask%d" % hp, [128, 8, 36]), biasc=_din(nc, "biasc%d" % hp, [128, 8, 8, 2]),
            wq=_din(nc, "wq%d" % hp, [1024, 512]), wkv=_din(nc, "wkv%d" % hp, [1024, 256])))
    shared = dict(Esel=_din(nc, "Esel", [64, 32, 128], BF16), addT=_din(nc, "addT", [32, 128, 64]),
                  overlap=_din(nc, "overlap", [256, 64], BF16), Emoba=_din(nc, "Emoba", [16, 32, 128], BF16),
                  addM=_din(nc, "addM", [32, 128, 16]), own01=_din(nc, "own01", [32, 128, 16]),
                  sele=_din(nc, "sele", [32, 32, 128], BF16))
    ln_mix = _din(nc, "ln_mix_all", [2, 1024])
    ln_ffn = _din(nc, "ln_ffn_all", [2, 1024])
    ln_ple = _din(nc, "ln_ple_all", [2, 1024])
    pin = _din(nc, "p", [2, 4096, 256])
    kv_norm = _din(nc, "kv_norm", [1024])
    bq_norm = _din(nc, "b_q_norm", [64])
    ks_norm = _din(nc, "k_norm_shared", [64])
    w_out = [_din(nc, "w_out%d" % L, [1024, 1024]) for L in range(2)]
    moe = dict(w_group=_din(nc, "moe_w_group", [2, 1024, 4]), b_group=_din(nc, "moe_b_group", [2, 4]),
               w_expert=_din(nc, "moe_w_expert", [2, 1024, 32]), b_expert=_din(nc, "moe_b_expert", [2, 32]),
               w_gate=_din(nc, "moe_w_gate", [2, 32, 1024, 128]), w_up=_din(nc, "moe_w_up", [2, 32, 1024, 128]),
               w_down=_din(nc, "moe_w_down", [2, 32, 128, 1024]), ple_w_proj=_din(nc, "ple_w_proj", [2, 256, 1024]),
               ple_w_gate=_din(nc, "ple_w_gate", [2, 1024, 1024]))
    o_s = [_dint(nc, "o_s%d" % L, [4096, 1024], BF16) for L in range(2)]
    h0_s = _dint(nc, "h0_s", [4096, 1024], F32)
    KT1 = _dint(nc, "KT1", [2, 64, 4096], BF16)
    V1 = _dint(nc, "V1", [2, 4096, 65], BF16)
    KM1 = _dint(nc, "KM1", [2, 64, 16], F32)
    hout = nc.dram_tensor("hout", [4096, 1024], F32, kind="ExternalOutput").ap()
    x = d0["x"]
    with ExitStack() as st:
        P = Prog(nc, st)
        C = Ctx(nc, P)

        def run_B(L, hin, hdst):
            for half in range(2):
                tk = slice(half * 2048, half * 2048 + 2048)
                dB = dict(hin=hin[tk, :], o=o_s[L][tk, :], p=pin[L, tk, :], w_out=w_out[L], ln_ffn=ln_ffn[L],
                          ln_ple=ln_ple[L], w_group=moe["w_group"][L], b_group=moe["b_group"][L],
                          w_expert=moe["w_expert"][L], b_expert=moe["b_expert"][L], w_gate=moe["w_gate"][L],
                          w_up=moe["w_up"][L], w_down=moe["w_down"][L], ple_w_proj=moe["ple_w_proj"][L],
                          ple_w_gate=moe["ple_w_gate"][L], identb=d0["identb"], identf=d0["identf"],
                          sele=shared["sele"], hout=hdst[tk, :])
                phase_B(C, 2048, dB)

        for hp in range(2):
            dA = dict(d0)
            dA.update(shared)
            dA.update(per_hp[hp])
            dA["ln_mix"] = ln_mix[0]
            dA["o"] = o_s[0][:, 512 * hp:512 * hp + 512]
            prep(C, dA, "nsa")
            attn_nsa(C, dA)
        run_B(0, x, h0_s)
        for hp in range(2):
            dA = dict(x=h0_s, ln_mix=ln_mix[1], kv_norm=kv_norm, q_norm=bq_norm, k_norm=ks_norm,
                      identb=d0["identb"], identf=d0["identf"], QT=d0["QT"], KT=KT1, V=V1, KM=KM1)
            dA.update(shared)
            dA.update(per_hp[hp])
            dA["o"] = o_s[1][:, 512 * hp:512 * hp + 512]
            prep(C, dA, "moba")
            attn_moba(C, dA)
        run_B(1, h0_s, hout)
        print("fused program instruction counts:", {e: len(q) for e, q in P.q.items()})
        P.emit()
    return nc


def kernel(**inputs):
    I = {k: np.ascontiguousarray(np.asarray(v)) for k, v in inputs.items()}
    cm = _consts_moba()
    sele = np.zeros((32, 32, 128), np.float32)
    for e in range(32):
        sele[e, e, :] = 1
    wkv = I['w_kv_shared']
    base = dict(ln_mix_all=I['ln_mix'], ln_ffn_all=I['ln_ffn'], ln_ple_all=I['ln_ple'], kv_norm=I['kv_norm'],
                b_q_norm=I['b_q_norm'][0], k_norm_shared=I['k_norm_shared'], w_out0=I['a_w_out'][0],
                w_out1=I['b_w_out'][0], moe_w_group=I['moe_w_group'], moe_b_group=I['moe_b_group'],
                moe_w_expert=I['moe_w_expert'], moe_b_expert=I['moe_b_expert'], moe_w_gate=I['moe_w_gate'],
                moe_w_up=I['moe_w_up'], moe_w_down=I['moe_w_down'], ple_w_proj=I['ple_w_proj'],
                ple_w_gate=I['ple_w_gate'], q_norm=I['a_q_norm'][0], k_norm=I['a_k_norm'][0],
                ck_pos=I['a_ck_pos'][0], ck_w1=I['a_ck_w1'][0], ck_w2=I['a_ck_w2'][0], cv_pos=I['a_cv_pos'][0],
                cv_w1=I['a_cv_w1'][0], cv_w2=I['a_cv_w2'][0], sele=sele.astype(_bf), **cm)
    cc = _consts_common(0)
    base["identb"], base["identf"] = cc["identb"], cc["identf"]
    for hp in range(2):
        cc = _consts_common(hp)
        cn = _consts_nsa(hp)
        base["wcat%d" % hp] = _wcat_for(I['a_w_in'][0], hp)
        base["qshift%d" % hp] = cc["qshift"]
        base["biask%d" % hp] = cc["biask"]
        base["biasc%d" % hp] = cn["biasc"]
        base["wq%d" % hp] = np.ascontiguousarray(I['b_w_q'][0][:, 512 * hp:512 * hp + 512])
        base["wkv%d" % hp] = np.ascontiguousarray(np.concatenate(
            [wkv[:, 128 * hp:128 * hp + 128], wkv[:, 256 + 128 * hp:256 + 128 * hp + 128]], 1))
        if hp == 0:
            base["Esel"], base["addT"], base["overlap"] = cn["Esel"], cn["addT"], cn["overlap"]
    maps = []
    for c in range(8):
        b = c // 2
        m = dict(base)
        m["x"] = I['x'][b]
        m["p"] = np.ascontiguousarray(I['p'][:, b])
        maps.append(m)
    res = run_bass_kernel_spmd(_build_fused(), maps, core_ids=list(range(8)))
    out = np.empty((4, 4096, 1024), np.float32)
    for c in range(8):
        b, hf = c // 2, c % 2
        out[b, hf * 2048:hf * 2048 + 2048] = res.results[c]["hout"][hf * 2048:hf * 2048 + 2048]
    return out
```

```python
from contextlib import ExitStack
import numpy as np
import concourse.bass as bass
import concourse.mybir as mybir


F32 = mybir.dt.float32
BF16 = mybir.dt.bfloat16
I32 = mybir.dt.int32
U32 = mybir.dt.uint32
ALU = mybir.AluOpType
AF = mybir.ActivationFunctionType
AX = mybir.AxisListType

ENGS = ("pe", "act", "dve", "pool", "sp")
N_DMA_SEMS = {"sp": 20, "pool": 12, "act": 2}


class Buf:
    __slots__ = ("name", "w", "r")

    def __init__(self, name="b"):
        self.name = name
        self.w = None
        self.r = {}


class Prog:
    def __init__(self, nc, stack):
        self.nc = nc
        self.q = {e: [] for e in ENGS}
        self.cnt = {}
        self.sems = {}
        self.seen = {e: {} for e in ENGS}
        for e in ("pe", "act", "dve", "pool"):
            k = "c_" + e
            self.sems[k] = stack.enter_context(nc.semaphore(k))
            self.cnt[k] = 0
        self.dma_pool = {}
        self.dma_rr = {}
        for e, n in N_DMA_SEMS.items():
            ks = []
            for i in range(n):
                k = "d_%s_%d" % (e, i)
                self.sems[k] = stack.enter_context(nc.semaphore(k))
                self.cnt[k] = 0
                ks.append(k)
            self.dma_pool[e] = ks
            self.dma_rr[e] = 0

    def _need(self, eng, deps):
        out = []
        seen = self.seen[eng]
        best = {}
        for d in deps:
            if d is None:
                continue
            k, v = d
            if best.get(k, 0) < v:
                best[k] = v
        for k, v in best.items():
            if seen.get(k, 0) < v:
                seen[k] = v
                out.append((k, v))
        return out

    def _deps(self, eng, reads, writes, own_key):
        deps = []
        for b in reads:
            deps.append(b.w)
        for b in writes:
            deps.append(b.w)
            for k, v in b.r.items():
                deps.append((k, v))
        if eng == "pe":
            deps = [d for d in deps if d is not None and d[0] != "c_pe"]
        return deps

    def op(self, eng, fn, reads=(), writes=()):
        key = "c_" + eng
        waits = self._need(eng, self._deps(eng, reads, writes, key))
        self.cnt[key] += 1
        val = self.cnt[key]
        self.q[eng].append((waits, fn, key, 1))
        for b in reads:
            if b.r.get(key, 0) < val:
                b.r[key] = val
        for b in writes:
            b.w = (key, val)
            b.r = {}
        return (key, val)

    def dma(self, eng, out_ap, in_ap, reads=(), writes=(), **kw):
        pool = self.dma_pool[eng]
        key = pool[self.dma_rr[eng] % len(pool)]
        self.dma_rr[eng] += 1
        deps = self._deps(eng, reads, writes, key)
        if self.cnt[key] > 0:
            deps.append((key, self.cnt[key]))
        waits = self._need(eng, deps)
        self.cnt[key] += 16
        val = self.cnt[key]

        def fn(e, out_ap=out_ap, in_ap=in_ap, kw=kw):
            return e.dma_start(out=out_ap, in_=in_ap, **kw)
        self.q[eng].append((waits, fn, key, 16))
        for b in reads:
            if b.r.get(key, 0) < val:
                b.r[key] = val
        for b in writes:
            b.w = (key, val)
            b.r = {}
        return (key, val)

    def barrier(self):
        allv = [(k, v) for k, v in self.cnt.items() if v > 0]
        for e in ENGS:
            waits = self._need(e, allv)
            if waits:
                self.q[e].append((waits, None, None, 0))

    def emit(self):
        nc = self.nc
        with nc.Block() as block:
            def run(engname):
                def body(e):
                    for waits, fn, key, inc in self.q[engname]:
                        for k, v in waits:
                            e.wait_ge(self.sems[k], v)
                        if fn is not None:
                            ins = fn(e)
                            ins.then_inc(self.sems[key], inc)
                return body
            block.tensor(run("pe"))
            block.scalar(run("act"))
            block.vector(run("dve"))
            block.gpsimd(run("pool"))
            block.sync(run("sp"))


class Tl:
    def __init__(self, t, name, nsub=0):
        self.t = t
        self.b = Buf(name)
        self.bs = [Buf("%s_%d" % (name, i)) for i in range(nsub)]

    def __getitem__(self, k):
        return self.t[k]


def _bufs(xs):
    out = []
    for x in xs:
        if isinstance(x, Tl):
            out.append(x.b)
        elif isinstance(x, Buf):
            out.append(x)
        elif isinstance(x, (list, tuple)):
            out.extend(_bufs(x))
        else:
            raise TypeError(x)
    return out


class Ctx:
    def __init__(self, nc, P):
        self.nc = nc
        self.P = P
        self.uid = 0

    def sb(self, st, shape, dt, name, nsub=0):
        self.uid += 1
        nm = "%s_%d" % (name, self.uid)
        return Tl(st.enter_context(self.nc.sbuf_tensor(nm, shape, dt)), nm, nsub)

    def ps(self, st, shape, dt, name, nsub=0):
        self.uid += 1
        nm = "%s_%d" % (name, self.uid)
        return Tl(st.enter_context(self.nc.psum_tensor(nm, shape, dt)), nm, nsub)

    def op(self, eng, fn, r=(), w=()):
        return self.P.op(eng, fn, _bufs(r), _bufs(w))

    def dma(self, eng, out_ap, in_ap, r=(), w=(), **kw):
        return self.P.dma(eng, out_ap, in_ap, _bufs(r), _bufs(w), **kw)


def _prog_custom16(self, eng, fn, reads=(), writes=()):
    pool = self.dma_pool[eng]
    key = pool[self.dma_rr[eng] % len(pool)]
    self.dma_rr[eng] += 1
    deps = self._deps(eng, reads, writes, key)
    if self.cnt[key] > 0:
        deps.append((key, self.cnt[key]))
    waits = self._need(eng, deps)
    self.cnt[key] += 16
    val = self.cnt[key]
    self.q[eng].append((waits, fn, key, 16))
    for b in reads:
        if b.r.get(key, 0) < val:
            b.r[key] = val
    for b in writes:
        b.w = (key, val)
        b.r = {}
    return (key, val)


Prog.custom16 = _prog_custom16


def rms_stats(C, src_ap, src_bufs, junk, ss, rstd, mhalf, width):
    C.op("act", lambda e: e.activation(out=junk[:, 0:width], in_=src_ap, func=AF.Square),
         r=src_bufs, w=[junk])
    C.op("dve", lambda e: e.reduce_sum(out=ss[:], in_=junk[:, 0:width], axis=AX.X), r=[junk], w=[ss])
    C.op("dve", lambda e: e.tensor_scalar(out=ss[:], in0=ss[:], scalar1=1.0 / width, scalar2=1e-6,
                                          op0=ALU.mult, op1=ALU.add), r=[ss], w=[ss])
    C.op("act", lambda e: e.activation(out=ss[:], in_=ss[:], func=AF.Sqrt), r=[ss], w=[ss])
    C.op("dve", lambda e: e.reciprocal(out=rstd[:], in_=ss[:]), r=[ss], w=[rstd])


def phase_B(C, NT, d):
    def mm_(out, lhsT, rhs, start, stop, r, w):
        C.op("pe", lambda e: e.matmul(out, lhsT=lhsT, rhs=rhs, start=start, stop=stop), r=r, w=w)

    def tr_(out, in_, ident, r, w):
        C.op("pe", lambda e: e.transpose(out=out, in_=in_, identity=ident), r=r, w=w)

    nc = C.nc
    NTT = NT // 128
    CH = 256
    NCH = NT // CH
    with ExitStack() as st:
        hres = C.sb(st, [128, NTT, 1024], F32, "hres", nsub=NTT)
        xn2T = C.sb(st, [128, 8, NT], BF16, "xn2T", nsub=NTT)
        cwT = C.sb(st, [32, NT], BF16, "cwT", nsub=NTT)
        identb = C.sb(st, [128, 128], BF16, "identb")
        identf = C.sb(st, [128, 128], F32, "identf")
        gffn = C.sb(st, [128, 1024], F32, "gffn")
        gple = C.sb(st, [128, 1024], F32, "gple")
        mhalf = C.sb(st, [128, 1], F32, "mhalf")
        sele = C.sb(st, [32, 32, 128], BF16, "sele")
        C.dma("sp", identb[:], d["identb"], w=[identb])
        C.dma("sp", identf[:], d["identf"], w=[identf])
        C.dma("sp", sele[:], d["sele"], w=[sele])
        C.dma("sp", gffn[:], d["ln_ffn"].partition_broadcast(128), w=[gffn])
        C.dma("sp", gple[:], d["ln_ple"].partition_broadcast(128), w=[gple])
        C.op("pool", lambda e: e.memset(mhalf[:], -0.5), w=[mhalf])

        with ExitStack() as s1:
            wo = C.sb(s1, [128, 8, 1024], BF16, "wo", nsub=8)
            wr = C.sb(s1, [128, 8, 36], F32, "wr")
            rbias = C.sb(s1, [128, 36], F32, "rbias")
            ot = [C.sb(s1, [128, 1024], BF16, "ot%d" % i) for i in range(3)]
            oT = [C.sb(s1, [128, 8, 128], BF16, "oT%d" % i) for i in range(2)]
            junk = [C.sb(s1, [128, 1024], F32, "junk%d" % i) for i in range(2)]
            xn2 = [C.sb(s1, [128, 1024], F32, "xn2%d" % i) for i in range(2)]
            xn2b = [C.sb(s1, [128, 1024], BF16, "xn2b%d" % i) for i in range(2)]
            xfT = [C.sb(s1, [128, 8, 128], F32, "xfT%d" % i) for i in range(2)]
            ss = [C.sb(s1, [128, 1], F32, "ss%d" % i) for i in range(2)]
            rstd = [C.sb(s1, [128, 1], F32, "rstd%d" % i) for i in range(2)]
            lg = [C.sb(s1, [128, 36], F32, "lg%d" % i) for i in range(2)]
            sm = [C.sb(s1, [128, 64], F32, "sm%d" % i) for i in range(2)]
            cw = [C.sb(s1, [128, 32], BF16, "cw%d" % i) for i in range(2)]
            pT = C.ps(s1, [128, 8, 128], BF16, "pT")
            pm = [C.ps(s1, [128, 512], F32, "pm%d" % i) for i in range(2)]
            pTf = C.ps(s1, [128, 8, 128], F32, "pTf")
            plg = [C.ps(s1, [128, 36], F32, "plg%d" % i) for i in range(2)]
            pcw = C.ps(s1, [32, 128], BF16, "pcw")

            for c in range(8):
                C.dma("pool", wo[:, c, :], d["w_out"][c * 128:(c + 1) * 128, :], w=[wo.bs[c]])
            C.dma("sp", wr[:, :, 0:4], d["w_group"].rearrange("(c p) n -> p c n", p=128), w=[wr])
            C.dma("sp", wr[:, :, 4:36], d["w_expert"].rearrange("(c p) n -> p c n", p=128), w=[wr])
            C.dma("sp", rbias[:, 0:4], d["b_group"].partition_broadcast(128), w=[rbias])
            C.dma("sp", rbias[:, 4:36], d["b_expert"].partition_broadcast(128), w=[rbias])

            def stA(t):
                b = t % 2
                tk = slice(t * 128, (t + 1) * 128)
                hb = hres.bs[t]
                o_t = ot[t % 3]
                C.dma("sp", hres[:, t, :], d["hin"][tk, :], w=[hb])
                C.dma("sp", o_t[:], d["o"][tk, :], w=[o_t])
                for c in range(8):
                    tr_(pT[:, c, :], o_t[:, c * 128:(c + 1) * 128], identb[:], [o_t, identb], [pT])
                C.op("act", lambda e: e.activation(out=oT[b][:], in_=pT[:], func=AF.Copy), r=[pT], w=[oT[b]])
                for hf in range(2):
                    for c in range(8):
                        mm_(pm[hf][:], oT[b][:, c, :], wo[:, c, hf * 512:(hf + 1) * 512], c == 0, c == 7,
                            [oT[b], wo.bs[c]], [pm[hf]])
                for hf in range(2):
                    hs = slice(hf * 512, (hf + 1) * 512)
                    C.op("dve", lambda e, hf=hf, hs=hs: e.tensor_tensor(out=hres[:, t, hs], in0=pm[hf][:],
                                                                         in1=hres[:, t, hs], op=ALU.add),
                         r=[pm[hf], hb], w=[hb])

            def stB(t):
                b = t % 2
                hb = hres.bs[t]
                rms_stats(C, hres[:, t, :], [hb], junk[b], ss[b], rstd[b], mhalf, 1024)
                C.op("dve", lambda e: e.scalar_tensor_tensor(out=xn2[b][:], in0=hres[:, t, :], scalar=rstd[b][:, 0:1],
                                                             in1=gffn[:], op0=ALU.mult, op1=ALU.mult),
                     r=[hb, rstd[b], gffn], w=[xn2[b]])
                C.op("act", lambda e: e.activation(out=xn2b[b][:], in_=xn2[b][:], func=AF.Copy), r=[xn2[b]],
                     w=[xn2b[b]])

            def stC(t):
                b = t % 2
                tk = slice(t * 128, (t + 1) * 128)
                for c in range(8):
                    tr_(pT[:, c, :], xn2b[b][:, c * 128:(c + 1) * 128], identb[:], [xn2b[b], identb], [pT])
                C.op("dve", lambda e: e.tensor_copy(out=xn2T[:, :, tk], in_=pT[:]), r=[pT], w=[xn2T.bs[t]])
                for c in range(8):
                    tr_(pTf[:, c, :], xn2[b][:, c * 128:(c + 1) * 128], identf[:], [xn2[b], identf], [pTf])
                C.op("act", lambda e: e.activation(out=xfT[b][:], in_=pTf[:], func=AF.Copy), r=[pTf], w=[xfT[b]])
                for c in range(8):
                    mm_(plg[b][:], xfT[b][:, c, :], wr[:, c, :], c == 0, c == 7, [xfT[b], wr], [plg[b]])

            def back(t):
                b = t % 2
                tk = slice(t * 128, (t + 1) * 128)
                lg_, sm_, cw_ = lg[b], sm[b], cw[b]
                C.op("dve", lambda e: e.tensor_tensor(out=lg_[:], in0=plg[b][:], in1=rbias[:], op=ALU.add),
                     r=[plg[b], rbias], w=[lg_])
                gmax, ngmax, gsum, gw = sm_[:, 0:1], sm_[:, 1:2], sm_[:, 2:3], sm_[:, 3:4]
                oh, ge = sm_[:, 4:8], sm_[:, 8:12]
                sel, m8 = sm_[:, 16:24], sm_[:, 24:32]
                nl1, e2, w1, c1, c2 = sm_[:, 32:33], sm_[:, 33:34], sm_[:, 34:35], sm_[:, 35:36], sm_[:, 36:37]
                ta, tb_ = sm_[:, 40:48], sm_[:, 48:56]

                def D(fn):
                    C.op("dve", fn, r=[lg_, sm_], w=[sm_])
                D(lambda e: e.reduce_max(out=gmax, in_=lg_[:, 0:4], axis=AX.X))
                D(lambda e: e.tensor_scalar(out=ngmax, in0=gmax, scalar1=-1.0, scalar2=None, op0=ALU.mult))
                C.op("act", lambda e: e.activation(out=ge, in_=lg_[:, 0:4], func=AF.Exp, bias=ngmax, scale=1.0),
                     r=[lg_, sm_], w=[sm_])
                D(lambda e: e.reduce_sum(out=gsum, in_=ge, axis=AX.X))
                D(lambda e: e.reciprocal(out=gw, in_=gsum))
                D(lambda e: e.tensor_scalar(out=oh, in0=lg_[:, 0:4], scalar1=gmax, scalar2=None, op0=ALU.is_equal))
                D(lambda e: e.tensor_scalar(out=sel, in0=lg_[:, 4:12], scalar1=oh[:, 0:1], scalar2=None, op0=ALU.mult))
                for g in range(1, 4):
                    D(lambda e, g=g: e.scalar_tensor_tensor(out=sel, in0=lg_[:, 4 + 8 * g:12 + 8 * g],
                                                             scalar=oh[:, g:g + 1], in1=sel,
                                                             op0=ALU.mult, op1=ALU.add))
                D(lambda e: e.max(out=m8, in_=sel))
                D(lambda e: e.tensor_scalar(out=nl1, in0=m8[:, 0:1], scalar1=-1.0, scalar2=None, op0=ALU.mult))
                C.op("act", lambda e: e.activation(out=e2, in_=m8[:, 1:2], func=AF.Exp, bias=nl1, scale=1.0),
                     r=[sm_], w=[sm_])
                D(lambda e: e.tensor_scalar(out=w1, in0=e2, scalar1=1.0, scalar2=None, op0=ALU.add))
                D(lambda e: e.reciprocal(out=w1, in_=w1))
                D(lambda e: e.tensor_tensor(out=c1, in0=w1, in1=gw, op=ALU.mult))
                D(lambda e: e.tensor_tensor(out=c2, in0=c1, in1=e2, op=ALU.mult))
                D(lambda e: e.tensor_scalar(out=ta, in0=sel, scalar1=m8[:, 0:1], scalar2=c1, op0=ALU.is_equal,
                                            op1=ALU.mult))
                D(lambda e: e.tensor_scalar(out=tb_, in0=sel, scalar1=m8[:, 1:2], scalar2=c2, op0=ALU.is_equal,
                                            op1=ALU.mult))
                D(lambda e: e.tensor_tensor(out=ta, in0=ta, in1=tb_, op=ALU.add))
                for g in range(4):
                    C.op("dve", lambda e, g=g: e.tensor_scalar(out=cw_[:, 8 * g:8 * g + 8], in0=ta,
                                                                scalar1=oh[:, g:g + 1], scalar2=None, op0=ALU.mult),
                         r=[sm_], w=[cw_])
                tr_(pcw[:], cw_[:], identb[:], [cw_, identb], [pcw])
                C.op("act", lambda e: e.activation(out=cwT[:, tk], in_=pcw[:], func=AF.Copy), r=[pcw], w=[cwT.bs[t]])

            stages = (stA, stB, stC, back)
            for step in range(NTT + len(stages) - 1):
                for si, fn in enumerate(stages):
                    t = step - si
                    if 0 <= t < NTT:
                        fn(t)
        C.P.barrier()
        if "dbg1" in d:
            for t in range(NTT):
                C.dma("sp", d["dbg1"][t * 128:(t + 1) * 128, :], hres[:, t, :], r=[hres.bs[t]])
            C.dma("sp", d["dbgcw"], cwT[:], r=cwT.bs)

        with ExitStack() as s2:
            GS = 4
            wg = [[C.sb(s2, [128, 8, 128], BF16, "wg%d_%d" % (i, j)) for j in range(GS)] for i in range(2)]
            wu = [[C.sb(s2, [128, 8, 128], BF16, "wu%d_%d" % (i, j)) for j in range(GS)] for i in range(2)]
            wd = [[C.sb(s2, [128, 1024], BF16, "wd%d_%d" % (i, j)) for j in range(GS)] for i in range(2)]
            sg = [C.sb(s2, [128, CH], BF16, "sg%d" % i) for i in range(2)]
            bcs = [C.sb(s2, [128, CH], BF16, "bcs%d" % i) for i in range(2)]
            tt = [C.sb(s2, [128, CH], BF16, "tt%d" % i) for i in range(2)]
            hid = [C.sb(s2, [128, CH], BF16, "hid%d" % i) for i in range(2)]
            pgu = [C.ps(s2, [128, 2, CH], F32, "pgu%d" % i) for i in range(2)]
            pbc = [C.ps(s2, [128, CH], F32, "pbc%d" % i) for i in range(2)]
            py = [[C.ps(s2, [128, 512], F32, "py%d_%d" % (s, hf)) for hf in range(2)] for s in range(2)]

            def load_group(G):
                par = G % 2
                for j in range(GS):
                    ex = G * GS + j
                    C.dma("pool", wg[par][j][:], d["w_gate"][ex].rearrange("(c p) f -> p c f", p=128), w=[wg[par][j]])
                    C.dma("pool", wu[par][j][:], d["w_up"][ex].rearrange("(c p) f -> p c f", p=128), w=[wu[par][j]])
                    C.dma("pool", wd[par][j][:], d["w_down"][ex], w=[wd[par][j]])

            def GU(G, ck, j, k):
                par = G % 2
                ex = G * GS + j
                cs = slice(ck * CH, (ck + 1) * CH)
                tb = [xn2T.bs[ck * (CH // 128) + i] for i in range(CH // 128)]
                for (which, wt) in ((0, wg[par][j]), (1, wu[par][j])):
                    for c in range(8):
                        C.op("pe", lambda e, c=c, wt=wt, which=which: e.matmul(pgu[k][:, which, :], lhsT=wt[:, c, :],
                                                                                rhs=xn2T[:, c, cs],
                                                                                start=(c == 0), stop=(c == 7)),
                             r=[wt] + tb, w=[pgu[k]])
                cb = [cwT.bs[ck * (CH // 128) + i] for i in range(CH // 128)]
                C.op("pe", lambda e: e.matmul(pbc[k][:], lhsT=sele[:, ex, :], rhs=cwT[:, cs], start=True, stop=True),
                     r=[sele] + cb, w=[pbc[k]])
                C.op("act", lambda e: e.activation(out=sg[k][:], in_=pgu[k][:, 0, :], func=AF.Silu),
                     r=[pgu[k]], w=[sg[k]])
                C.op("act", lambda e: e.activation(out=bcs[k][:], in_=pbc[k][:], func=AF.Copy), r=[pbc[k]], w=[bcs[k]])
                C.op("dve", lambda e: e.tensor_tensor(out=tt[k][:], in0=pgu[k][:, 1, :], in1=sg[k][:], op=ALU.mult),
                     r=[pgu[k], sg[k]], w=[tt[k]])
                C.op("dve", lambda e: e.tensor_tensor(out=hid[k][:], in0=tt[k][:], in1=bcs[k][:], op=ALU.mult),
                     r=[tt[k], bcs[k]], w=[hid[k]])
                if "dbg3" in d and G == 0 and ck == 0 and j == 1:
                    for i, tl in enumerate((sg[k], bcs[k], tt[k], hid[k])):
                        C.dma("sp", d["dbg3"][i], tl[:], r=[tl])
                    C.dma("sp", d["dbg3"][4][:, 0:128], wg[par][j][:, 3, :], r=[wg[par][j]])
                    C.dma("sp", d["dbg3"][5][:, 0:128], xn2T[:, 3, 0:128], r=tb)

            def DOWN(G, ck, j, k):
                par = G % 2
                for s in range(CH // 128):
                    for hf in range(2):
                        C.op("pe", lambda e, s=s, hf=hf: e.matmul(py[s][hf][:], lhsT=hid[k][:, s * 128:(s + 1) * 128],
                                                                   rhs=wd[par][j][:, hf * 512:(hf + 1) * 512],
                                                                   start=(j == 0), stop=(j == GS - 1)),
                             r=[hid[k], wd[par][j]], w=[py[s][hf]])

            NG = 32 // GS
            load_group(0)
            for G in range(NG):
                if G + 1 < NG:
                    load_group(G + 1)
                for ck in range(NCH):
                    GU(G, ck, 0, 0)
                    for j in range(1, GS):
                        GU(G, ck, j, j % 2)
                        DOWN(G, ck, j - 1, (j - 1) % 2)
                    DOWN(G, ck, GS - 1, (GS - 1) % 2)
                    for s in range(CH // 128):
                        t = ck * (CH // 128) + s
                        for hf in range(2):
                            C.op("dve", lambda e, s=s, hf=hf, t=t: e.tensor_tensor(
                                out=hres[:, t, hf * 512:(hf + 1) * 512], in0=py[s][hf][:],
                                in1=hres[:, t, hf * 512:(hf + 1) * 512], op=ALU.add),
                                r=[py[s][hf], hres.bs[t]], w=[hres.bs[t]])
        C.P.barrier()
        if "dbg2" in d:
            for t in range(NTT):
                C.dma("sp", d["dbg2"][t * 128:(t + 1) * 128, :], hres[:, t, :], r=[hres.bs[t]])

        with ExitStack() as s3:
            wpg = C.sb(s3, [128, 8, 1024], BF16, "wpg", nsub=8)
            wpp = C.sb(s3, [128, 2, 1024], BF16, "wpp", nsub=2)
            junk_3 = C.sb(s3, [128, 1024], F32, "junk3")
            ss_3 = C.sb(s3, [128, 1], F32, "ss3")
            rstd_3 = C.sb(s3, [128, 1], F32, "rstd3")
            xn3 = C.sb(s3, [128, 1024], BF16, "xn3")
            x3T = C.sb(s3, [128, 8, 128], BF16, "x3T")
            pt = [C.sb(s3, [128, 256], F32, "pt%d" % i) for i in range(2)]
            ptb = C.sb(s3, [128, 256], BF16, "ptb")
            ppT = C.sb(s3, [128, 2, 128], BF16, "ppT")
            sig = C.sb(s3, [128, 1024], F32, "sig")
            gp = C.sb(s3, [128, 1024], F32, "gp")
            ho = [C.sb(s3, [128, 1024], F32, "ho%d" % i) for i in range(2)]
            pT_3 = C.ps(s3, [128, 8, 128], BF16, "pT3")
            pT2 = C.ps(s3, [128, 2, 128], BF16, "pT23")
            pg = [C.ps(s3, [128, 512], F32, "pg%d" % i) for i in range(2)]
            pq = [C.ps(s3, [128, 512], F32, "pq%d" % i) for i in range(2)]
            for c in range(8):
                C.dma("pool", wpg[:, c, :], d["ple_w_gate"][c * 128:(c + 1) * 128, :], w=[wpg.bs[c]])
            for c in range(2):
                C.dma("pool", wpp[:, c, :], d["ple_w_proj"][c * 128:(c + 1) * 128, :], w=[wpp.bs[c]])
            for t in range(NTT):
                tk = slice(t * 128, (t + 1) * 128)
                hb = hres.bs[t]
                p_t = pt[t % 2]
                h_o = ho[t % 2]
                C.dma("sp", p_t[:], d["p"][tk, :], w=[p_t])
                rms_stats(C, hres[:, t, :], [hb], junk_3, ss_3, rstd_3, mhalf, 1024)
                C.op("dve", lambda e, t=t: e.scalar_tensor_tensor(out=xn3[:], in0=hres[:, t, :], scalar=rstd_3[:, 0:1],
                                                                   in1=gple[:], op0=ALU.mult, op1=ALU.mult),
                     r=[hb, rstd_3, gple], w=[xn3])
                for c in range(8):
                    C.op("pe", lambda e, c=c: e.transpose(out=pT_3[:, c, :], in_=xn3[:, c * 128:(c + 1) * 128],
                                                           identity=identb[:]), r=[xn3, identb], w=[pT_3])
                C.op("act", lambda e: e.activation(out=x3T[:], in_=pT_3[:], func=AF.Copy), r=[pT_3], w=[x3T])
                C.op("pool", lambda e, p_t=p_t: e.tensor_copy(out=ptb[:], in_=p_t[:]), r=[p_t], w=[ptb])
                for c in range(2):
                    C.op("pe", lambda e, c=c: e.transpose(out=pT2[:, c, :], in_=ptb[:, c * 128:(c + 1) * 128],
                                                           identity=identb[:]), r=[ptb, identb], w=[pT2])
                C.op("dve", lambda e: e.tensor_copy(out=ppT[:], in_=pT2[:]), r=[pT2], w=[ppT])
                for hf in range(2):
                    for c in range(8):
                        C.op("pe", lambda e, c=c, hf=hf: e.matmul(pg[hf][:], lhsT=x3T[:, c, :],
                                                                   rhs=wpg[:, c, hf * 512:(hf + 1) * 512],
                                                                   start=(c == 0), stop=(c == 7)),
                             r=[x3T, wpg.bs[c]], w=[pg[hf]])
                    for c in range(2):
                        C.op("pe", lambda e, c=c, hf=hf: e.matmul(pq[hf][:], lhsT=ppT[:, c, :],
                                                                   rhs=wpp[:, c, hf * 512:(hf + 1) * 512],
                                                                   start=(c == 0), stop=(c == 1)),
                             r=[ppT, wpp.bs[c]], w=[pq[hf]])
                for hf in range(2):
                    hs = slice(hf * 512, (hf + 1) * 512)
                    C.op("act", lambda e, hf=hf, hs=hs: e.activation(out=sig[:, hs], in_=pg[hf][:], func=AF.Sigmoid),
                         r=[pg[hf]], w=[sig])
                    C.op("dve", lambda e, hf=hf, hs=hs: e.tensor_tensor(out=gp[:, hs], in0=pq[hf][:], in1=sig[:, hs],
                                                                         op=ALU.mult), r=[pq[hf], sig], w=[gp])
                C.op("pool", lambda e, t=t, h_o=h_o: e.tensor_tensor(out=h_o[:], in0=gp[:], in1=hres[:, t, :],
                                                                      op=ALU.add), r=[gp, hb], w=[h_o])
                C.dma("pool", d["hout"][tk, :], h_o[:], r=[h_o])
        C.P.barrier()


NEG = -30000.0


def mm(C, out, lhsT, rhs, start, stop, r, w):
    C.op("pe", lambda e: e.matmul(out, lhsT=lhsT, rhs=rhs, start=start, stop=stop), r=r, w=w)


def tr(C, out, in_, ident, r, w):
    C.op("pe", lambda e: e.transpose(out=out, in_=in_, identity=ident), r=r, w=w)


def act(C, out, in_, func, r, w, bias=None, scale=1.0):
    if bias is None:
        C.op("act", lambda e: e.activation(out=out, in_=in_, func=func, scale=scale), r=r, w=w)
    else:
        C.op("act", lambda e: e.activation(out=out, in_=in_, func=func, bias=bias, scale=scale), r=r, w=w)


def tt(C, eng, out, in0, in1, op, r, w):
    C.op(eng, lambda e: e.tensor_tensor(out=out, in0=in0, in1=in1, op=op), r=r, w=w)


def ts(C, eng, out, in0, s1, s2, op0, op1, r, w):
    if s2 is None:
        C.op(eng, lambda e: e.tensor_scalar(out=out, in0=in0, scalar1=s1, scalar2=None, op0=op0), r=r, w=w)
    else:
        C.op(eng, lambda e: e.tensor_scalar(out=out, in0=in0, scalar1=s1, scalar2=s2, op0=op0, op1=op1), r=r, w=w)


def stt(C, out, in0, scalar, in1, op0, op1, r, w):
    C.op("dve", lambda e: e.scalar_tensor_tensor(out=out, in0=in0, scalar=scalar, in1=in1, op0=op0, op1=op1),
         r=r, w=w)


def cp(C, eng, out, in_, r, w):
    C.op(eng, lambda e: e.tensor_copy(out=out, in_=in_), r=r, w=w)


def memset(C, eng, ap, val, w):
    C.op(eng, lambda e: e.memset(ap, val), w=w)


def asel(C, out, in_, pattern, base, cm, r, w):
    C.op("pool", lambda e: e.affine_select(out=out, in_=in_, pattern=pattern, compare_op=ALU.is_ge, fill=0.0,
                                           base=base, channel_multiplier=cm), r=r, w=w)


def rsqrt_chain(C, ssq, n_inv, r, w):
    ts(C, "dve", ssq, ssq, n_inv, 1e-6, ALU.mult, ALU.add, r=r, w=w)
    act(C, ssq, ssq, AF.Sqrt, r=r, w=w)
    C.op("dve", lambda e: e.reciprocal(out=ssq, in_=ssq), r=r, w=w)


def headnorm(C, src, n, gain_t, scale, sq, ssq, outs, srcb):
    sqv = sq[:, 0:n * 64]
    act(C, sqv, src, AF.Square, r=[srcb], w=[sq])
    C.op("dve", lambda e: e.tensor_reduce(out=ssq[:, 0:n], in_=sq[:, 0:n * 64].rearrange("p (n d) -> p n d", d=64),
                                          axis=AX.X, op=ALU.add), r=[sq], w=[ssq])
    ts(C, "dve", ssq[:, 0:n], ssq[:, 0:n], 1.0 / 64, 1e-6, ALU.mult, ALU.add, r=[ssq], w=[ssq])
    act(C, ssq[:, 0:n], ssq[:, 0:n], AF.Sqrt, r=[ssq], w=[ssq])
    C.op("dve", lambda e: e.reciprocal(out=ssq[:, 0:n], in_=ssq[:, 0:n]), r=[ssq], w=[ssq])
    if scale != 1.0:
        ts(C, "dve", ssq[:, 0:n], ssq[:, 0:n], float(scale), None, ALU.mult, None, r=[ssq], w=[ssq])
    s3 = src.rearrange("p (n d) -> p n d", d=64)
    q3 = sq[:, 0:n * 64].rearrange("p (n d) -> p n d", d=64)
    rb = ssq[:, 0:n].unsqueeze(2).to_broadcast([128, n, 64])
    tt(C, "dve", q3, s3, rb, ALU.mult, r=[srcb, ssq, sq], w=[sq])
    g3 = gain_t[:, 0:n * 64].rearrange("p (n d) -> p n d", d=64)
    for (o_ap, o_t) in outs:
        tt(C, "dve", o_ap.rearrange("p (n d) -> p n d", d=64), q3, g3, ALU.mult, r=[sq, gain_t], w=[o_t])


def prep(C, d, kind):
    nsa = (kind == "nsa")
    NW = 1304 if nsa else 768
    with ExitStack() as st:
        identb = C.sb(st, [128, 128], BF16, "identb")
        C.dma("sp", identb[:], d["identb"], w=[identb])
        g1 = C.sb(st, [128, 1024], F32, "g1")
        C.dma("sp", g1[:], d["ln_mix"].partition_broadcast(128), w=[g1])
        if not nsa:
            g2 = C.sb(st, [128, 1024], F32, "g2")
            C.dma("sp", g2[:], d["kv_norm"].partition_broadcast(128), w=[g2])
        wb = C.sb(st, [128, 8, NW], BF16, "wb", nsub=8)
        for c in range(8):
            if nsa:
                C.dma("pool", wb[:, c, 0:512], d["wcat"][c * 128:(c + 1) * 128, 0:512], w=[wb.bs[c]])
                C.dma("pool", wb[:, c, 512:1024], d["wcat"][c * 128:(c + 1) * 128, 512:1024], w=[wb.bs[c]])
                C.dma("pool", wb[:, c, 1024:1304], d["wcat"][c * 128:(c + 1) * 128, 1024:1304], w=[wb.bs[c]])
            else:
                C.dma("pool", wb[:, c, 0:512], d["wq"][c * 128:(c + 1) * 128, :], w=[wb.bs[c]])
                C.dma("pool", wb[:, c, 512:768], d["wkv"][c * 128:(c + 1) * 128, :], w=[wb.bs[c]])
        gq = C.sb(st, [128, 512], F32, "gq")
        for h in range(8):
            C.dma("sp", gq[:, h * 64:(h + 1) * 64], d["q_norm"].partition_broadcast(128), w=[gq])
        gk = C.sb(st, [128, 256], F32, "gk")
        if nsa:
            for j, row in enumerate((1, 1, 2, 2)):
                C.dma("sp", gk[:, j * 64:(j + 1) * 64], d["k_norm"][row].partition_broadcast(128), w=[gk])
        else:
            for j in range(2):
                C.dma("sp", gk[:, j * 64:(j + 1) * 64], d["k_norm"].partition_broadcast(128), w=[gk])
            ones256 = C.sb(st, [128, 1], F32, "ones256")
            memset(C, "pool", ones256[:], 1.0 / 256, w=[ones256])
            kmT = C.sb(st, [64, 2, 16], F32, "kmT")
        two = range(2)
        xt = [C.sb(st, [128, 1024], F32, "xt%d" % i) for i in range(3)]
        junk = [C.sb(st, [128, 1024], F32, "junk%d" % i) for i in two]
        ss = [C.sb(st, [128, 1], F32, "ss%d" % i) for i in two]
        xn = [C.sb(st, [128, 1024], BF16, "xn%d" % i) for i in two]
        xT = [C.sb(st, [128, 8, 128], BF16, "xT%d" % i) for i in two]
        xn2 = [C.sb(st, [128, 1024], BF16, "xn2%d" % i) for i in two]
        xT2 = [C.sb(st, [128, 8, 128], BF16, "xT2%d" % i) for i in two]
        sq = [C.sb(st, [128, 512], F32, "sq%d" % i) for i in two]
        ssq = [C.sb(st, [128, 8], F32, "ssq%d" % i) for i in two]
        sqk = [C.sb(st, [128, 512], F32, "sqk%d" % i) for i in two]
        ssqk = [C.sb(st, [128, 8], F32, "ssqk%d" % i) for i in two]
        qn = [C.sb(st, [128, 512], BF16, "qn%d" % i) for i in two]
        kn = [C.sb(st, [128, 256], BF16, "kn%d" % i) for i in two]
        knf = [C.sb(st, [128, 256], F32, "knf%d" % i) for i in two]
        raw = [C.sb(st, [128, 256], BF16, "raw%d" % i) for i in two]
        qTs = [C.sb(st, [64, 8, 128], BF16, "qTs%d" % i) for i in two]
        kTs = [C.sb(st, [64, 8, 128], BF16, "kTs%d" % i) for i in two]
        va = [C.sb(st, [128, 4, 65], BF16, "va%d" % i) for i in two]
        gts = [C.sb(st, [128, 24], F32, "gts%d" % i) for i in two]
        for i in two:
            memset(C, "pool", va[i][:], 1.0, w=[va[i]])
        pT = C.ps(st, [128, 8, 128], BF16, "pT")
        pq = [C.ps(st, [128, 512], F32, "pq%d" % i) for i in two]
        pk = [C.ps(st, [128, 512], F32, "pk%d" % i) for i in two]
        if nsa:
            pv = [C.ps(st, [128, 512], F32, "pv%d" % i) for i in two]
        else:
            pkm = [C.ps(st, [64, 512], F32, "pkm%d" % i) for i in two]
        pT2 = C.ps(st, [64, 8, 128], BF16, "pT2")

        def front(t):
            b = t % 2
            tk = slice(t * 128, (t + 1) * 128)
            x_t = xt[t % 3]
            C.dma("sp", x_t[:], d["x"][tk, :], w=[x_t])
            act(C, junk[b][:], x_t[:], AF.Square, r=[x_t], w=[junk[b]])
            C.op("dve", lambda e: e.reduce_sum(out=ss[b][:], in_=junk[b][:], axis=AX.X), r=[junk[b]], w=[ss[b]])
            rsqrt_chain(C, ss[b][:], 1.0 / 1024, r=[ss[b]], w=[ss[b]])
            stt(C, xn[b][:], x_t[:], ss[b][:, 0:1], g1[:], ALU.mult, ALU.mult, r=[x_t, ss[b], g1], w=[xn[b]])
            if not nsa:
                stt(C, xn2[b][:], x_t[:], ss[b][:, 0:1], g2[:], ALU.mult, ALU.mult, r=[x_t, ss[b], g2], w=[xn2[b]])

        def mid(t):
            b = t % 2
            for c in range(8):
                tr(C, pT[:, c, :], xn[b][:, c * 128:(c + 1) * 128], identb[:], r=[xn[b], identb], w=[pT])
            act(C, xT[b][:], pT[:], AF.Copy, r=[pT], w=[xT[b]])
            xTk = xT[b]
            if not nsa:
                for c in range(8):
                    tr(C, pT[:, c, :], xn2[b][:, c * 128:(c + 1) * 128], identb[:], r=[xn2[b], identb], w=[pT])
                cp(C, "dve", xT2[b][:], pT[:], r=[pT], w=[xT2[b]])
                xTk = xT2[b]
            for c in range(8):
                mm(C, pq[b][:], xT[b][:, c, :], wb[:, c, 0:512], c == 0, c == 7, r=[xT[b], wb.bs[c]], w=[pq[b]])
            if nsa:
                for c in range(8):
                    mm(C, pk[b][:], xT[b][:, c, :], wb[:, c, 512:1024], c == 0, c == 7, r=[xT[b], wb.bs[c]],
                       w=[pk[b]])
                for c in range(8):
                    mm(C, pv[b][:, 0:280], xT[b][:, c, :], wb[:, c, 1024:1304], c == 0, c == 7,
                       r=[xT[b], wb.bs[c]], w=[pv[b]])
            else:
                for c in range(8):
                    mm(C, pk[b][:, 0:256], xTk[:, c, :], wb[:, c, 512:768], c == 0, c == 7, r=[xTk, wb.bs[c]],
                       w=[pk[b]])

        def back(t):
            b = t % 2
            tk = slice(t * 128, (t + 1) * 128)
            headnorm(C, pq[b][:], 8, gq, 0.125, sq[b], ssq[b], [(qn[b][:], qn[b])], pq[b])
            q_s = qTs[b]
            for h in range(8):
                tr(C, pT2[:, h, :], qn[b][:, h * 64:(h + 1) * 64], identb[:], r=[qn[b], identb], w=[pT2])
            act(C, q_s[:], pT2[:], AF.Copy, r=[pT2], w=[q_s])
            C.dma("pool", d["QT"][:, 0:64, tk].rearrange("h d t -> d h t"), q_s[:], r=[q_s])
            k_s = kTs[b]
            v_a = va[b]
            if nsa:
                headnorm(C, pk[b][:, 0:256], 4, gk, 1.0, sqk[b], ssqk[b], [(kn[b][:], kn[b])], pk[b])
                act(C, raw[b][:], pk[b][:, 256:512], AF.Copy, r=[pk[b]], w=[raw[b]])
                for j in range(4):
                    tr(C, pT2[:, j, :], kn[b][:, j * 64:(j + 1) * 64], identb[:], r=[kn[b], identb], w=[pT2])
                for j in range(4):
                    tr(C, pT2[:, 4 + j, :], raw[b][:, j * 64:(j + 1) * 64], identb[:], r=[raw[b], identb], w=[pT2])
                cp(C, "dve", k_s[:], pT2[:], r=[pT2], w=[k_s])
                C.dma("sp", d["KT"][:, 0:64, tk].rearrange("h d t -> d h t"), k_s[:], r=[k_s])
                act(C, v_a[:, :, 0:64], pv[b][:, 0:256].rearrange("p (j d) -> p j d", d=64), AF.Copy, r=[pv[b]],
                    w=[v_a])
                C.dma("sp", d["V"][:, tk, :].rearrange("j t d -> t j d"), v_a[:], r=[v_a])
                g_s = gts[b]
                act(C, g_s[:], pv[b][:, 256:280], AF.Sigmoid, r=[pv[b]], w=[g_s])
                C.dma("sp", d["G"][tk, :], g_s[:], r=[g_s])
            else:
                headnorm(C, pk[b][:, 0:128], 2, gk, 1.0, sqk[b], ssqk[b],
                         [(kn[b][:, 0:128], kn[b]), (knf[b][:, 0:128], knf[b])], pk[b])
                for j in range(2):
                    tr(C, pT2[:, j, :], kn[b][:, j * 64:(j + 1) * 64], identb[:], r=[kn[b], identb], w=[pT2])
                cp(C, "dve", k_s[:, 0:2, :], pT2[:, 0:2, :], r=[pT2], w=[k_s])
                C.dma("pool", d["KT"][:, 0:64, tk].rearrange("h d t -> d h t"), k_s[:, 0:2, :], r=[k_s])
                act(C, v_a[:, 0:2, 0:64], pk[b][:, 128:256].rearrange("p (j d) -> p j d", d=64), AF.Copy,
                    r=[pk[b]], w=[v_a])
                C.dma("pool", d["V"][:, tk, :].rearrange("j t d -> t j d"), v_a[:, 0:2, :], r=[v_a])
                for j in range(2):
                    mm(C, pkm[j][:, 0:1], knf[b][:, j * 64:(j + 1) * 64], ones256[:], t % 2 == 0, t % 2 == 1,
                       r=[knf[b], ones256], w=[pkm[j]])
                if t % 2 == 1:
                    for j in range(2):
                        cp(C, "dve", kmT[:, j, t // 2:t // 2 + 1], pkm[j][:, 0:1], r=[pkm[j]], w=[kmT])

        stages = (front, mid, back)
        for step in range(32 + len(stages) - 1):
            for si, fn in enumerate(stages):
                t = step - si
                if 0 <= t < 32:
                    fn(t)
        if not nsa:
            C.dma("sp", d["KM"].rearrange("g d n -> d g n"), kmT[:], r=[kmT])
    C.P.barrier()


def prep_v1(C, d, kind):
    nc = C.nc
    nsa = (kind == "nsa")
    NW = 1304 if nsa else 768
    with ExitStack() as st:
        identb = C.sb(st, [128, 128], BF16, "identb")
        identf = C.sb(st, [128, 128], F32, "identf")
        C.dma("sp", identb[:], d["identb"], w=[identb])
        C.dma("sp", identf[:], d["identf"], w=[identf])
        g1 = C.sb(st, [128, 1024], F32, "g1")
        C.dma("sp", g1[:], d["ln_mix"].partition_broadcast(128), w=[g1])
        if not nsa:
            g2 = C.sb(st, [128, 1024], F32, "g2")
            C.dma("sp", g2[:], d["kv_norm"].partition_broadcast(128), w=[g2])
        wb = C.sb(st, [128, 8, NW], BF16, "wb", nsub=8)
        wst = [C.sb(st, [128, 1304], F32, "wst%d" % i) for i in range(2)]
        for c in range(8):
            w_s = wst[c % 2]
            if nsa:
                C.dma("sp", w_s[:, 0:NW], d["wcat"][c * 128:(c + 1) * 128, :], w=[w_s])
            else:
                C.dma("sp", w_s[:, 0:512], d["wq"][c * 128:(c + 1) * 128, :], w=[w_s])
                C.dma("sp", w_s[:, 512:768], d["wkv"][c * 128:(c + 1) * 128, :], w=[w_s])
            cp(C, "pool", wb[:, c, :], w_s[:, 0:NW], r=[w_s], w=[wb.bs[c]])
        gq = C.sb(st, [128, 512], F32, "gq")
        for h in range(8):
            C.dma("sp", gq[:, h * 64:(h + 1) * 64], d["q_norm"].partition_broadcast(128), w=[gq])
        gk = C.sb(st, [128, 256], F32, "gk")
        if nsa:
            for j, row in enumerate((1, 1, 2, 2)):
                C.dma("sp", gk[:, j * 64:(j + 1) * 64], d["k_norm"][row].partition_broadcast(128), w=[gk])
        else:
            for j in range(2):
                C.dma("sp", gk[:, j * 64:(j + 1) * 64], d["k_norm"].partition_broadcast(128), w=[gk])
            ones256 = C.sb(st, [128, 1], F32, "ones256")
            memset(C, "pool", ones256[:], 1.0 / 256, w=[ones256])
            kmT = C.sb(st, [64, 2, 16], F32, "kmT")
        xt = [C.sb(st, [128, 1024], F32, "xt%d" % i) for i in range(2)]
        junk = C.sb(st, [128, 1024], F32, "junk")
        ss = C.sb(st, [128, 1], F32, "ss")
        xn = C.sb(st, [128, 1024], BF16, "xn")
        xT = C.sb(st, [128, 8, 128], BF16, "xT")
        xn2 = C.sb(st, [128, 1024], BF16, "xn2")
        xT2 = C.sb(st, [128, 8, 128], BF16, "xT2")
        sq = C.sb(st, [128, 512], F32, "sq")
        ssq = C.sb(st, [128, 8], F32, "ssq")
        qn = C.sb(st, [128, 512], BF16, "qn")
        kn = C.sb(st, [128, 256], BF16, "kn")
        knf = C.sb(st, [128, 256], F32, "knf")
        raw = C.sb(st, [128, 256], BF16, "raw")
        qTs = [C.sb(st, [64, 8, 128], BF16, "qTs%d" % i) for i in range(2)]
        kTs = [C.sb(st, [64, 8, 128], BF16, "kTs%d" % i) for i in range(2)]
        va = [C.sb(st, [128, 4, 65], BF16, "va%d" % i) for i in range(2)]
        gts = [C.sb(st, [128, 24], F32, "gts%d" % i) for i in range(2)]
        for i in range(2):
            memset(C, "pool", va[i][:], 1.0, w=[va[i]])
        pT = C.ps(st, [128, 8, 128], BF16, "pT")
        pq = C.ps(st, [128, 512], F32, "pq")
        pk = C.ps(st, [128, 512], F32, "pk")
        pv = C.ps(st, [128, 512], F32, "pv")
        pT2 = C.ps(st, [64, 8, 128], BF16, "pT2")
        pkm = [C.ps(st, [64, 512], F32, "pkm%d" % i) for i in range(2)]

        for t in range(32):
            tk = slice(t * 128, (t + 1) * 128)
            x_t = xt[t % 2]
            C.dma("sp", x_t[:], d["x"][tk, :], w=[x_t])
            act(C, junk[:], x_t[:], AF.Square, r=[x_t], w=[junk])
            C.op("dve", lambda e: e.reduce_sum(out=ss[:], in_=junk[:], axis=AX.X), r=[junk], w=[ss])
            rsqrt_chain(C, ss[:], 1.0 / 1024, r=[ss], w=[ss])
            stt(C, xn[:], x_t[:], ss[:, 0:1], g1[:], ALU.mult, ALU.mult, r=[x_t, ss, g1], w=[xn])
            for c in range(8):
                tr(C, pT[:, c, :], xn[:, c * 128:(c + 1) * 128], identb[:], r=[xn, identb], w=[pT])
            act(C, xT[:], pT[:], AF.Copy, r=[pT], w=[xT])
            if nsa:
                xTk = xT
            else:
                stt(C, xn2[:], x_t[:], ss[:, 0:1], g2[:], ALU.mult, ALU.mult, r=[x_t, ss, g2], w=[xn2])
                for c in range(8):
                    tr(C, pT[:, c, :], xn2[:, c * 128:(c + 1) * 128], identb[:], r=[xn2, identb], w=[pT])
                cp(C, "dve", xT2[:], pT[:], r=[pT], w=[xT2])
                xTk = xT2
            for c in range(8):
                mm(C, pq[:], xT[:, c, :], wb[:, c, 0:512], c == 0, c == 7, r=[xT, wb.bs[c]], w=[pq])
            if nsa:
                for c in range(8):
                    mm(C, pk[:], xT[:, c, :], wb[:, c, 512:1024], c == 0, c == 7, r=[xT, wb.bs[c]], w=[pk])
                for c in range(8):
                    mm(C, pv[:, 0:280], xT[:, c, :], wb[:, c, 1024:1304], c == 0, c == 7, r=[xT, wb.bs[c]], w=[pv])
            else:
                for c in range(8):
                    mm(C, pk[:, 0:256], xTk[:, c, :], wb[:, c, 512:768], c == 0, c == 7, r=[xTk, wb.bs[c]], w=[pk])
            headnorm(C, pq[:], 8, gq, 0.125, sq, ssq, [(qn[:], qn)], pq)
            q_s = qTs[t % 2]
            for h in range(8):
                tr(C, pT2[:, h, :], qn[:, h * 64:(h + 1) * 64], identb[:], r=[qn, identb], w=[pT2])
            act(C, q_s[:], pT2[:], AF.Copy, r=[pT2], w=[q_s])
            C.dma("sp", d["QT"][:, 0:64, tk].rearrange("h d t -> d h t"), q_s[:], r=[q_s])
            k_s = kTs[t % 2]
            v_a = va[t % 2]
            if nsa:
                headnorm(C, pk[:, 0:256], 4, gk, 1.0, sq, ssq, [(kn[:], kn)], pk)
                act(C, raw[:], pk[:, 256:512], AF.Copy, r=[pk], w=[raw])
                for j in range(4):
                    tr(C, pT2[:, j, :], kn[:, j * 64:(j + 1) * 64], identb[:], r=[kn, identb], w=[pT2])
                for j in range(4):
                    tr(C, pT2[:, 4 + j, :], raw[:, j * 64:(j + 1) * 64], identb[:], r=[raw, identb], w=[pT2])
                cp(C, "dve", k_s[:], pT2[:], r=[pT2], w=[k_s])
                C.dma("sp", d["KT"][:, 0:64, tk].rearrange("h d t -> d h t"), k_s[:], r=[k_s])
                act(C, v_a[:, :, 0:64], pv[:, 0:256].rearrange("p (j d) -> p j d", d=64), AF.Copy, r=[pv], w=[v_a])
                C.dma("sp", d["V"][:, tk, :].rearrange("j t d -> t j d"), v_a[:], r=[v_a])
                g_s = gts[t % 2]
                act(C, g_s[:], pv[:, 256:280], AF.Sigmoid, r=[pv], w=[g_s])
                C.dma("sp", d["G"][tk, :], g_s[:], r=[g_s])
            else:
                headnorm(C, pk[:, 0:128], 2, gk, 1.0, sq, ssq, [(kn[:, 0:128], kn), (knf[:, 0:128], knf)], pk)
                for j in range(2):
                    tr(C, pT2[:, j, :], kn[:, j * 64:(j + 1) * 64], identb[:], r=[kn, identb], w=[pT2])
                cp(C, "dve", k_s[:, 0:2, :], pT2[:, 0:2, :], r=[pT2], w=[k_s])
                C.dma("sp", d["KT"][:, 0:64, tk].rearrange("h d t -> d h t"), k_s[:, 0:2, :], r=[k_s])
                act(C, v_a[:, 0:2, 0:64], pk[:, 128:256].rearrange("p (j d) -> p j d", d=64), AF.Copy, r=[pk], w=[v_a])
                C.dma("sp", d["V"][:, tk, :].rearrange("j t d -> t j d"), v_a[:, 0:2, :], r=[v_a])
                for j in range(2):
                    mm(C, pkm[j][:, 0:1], knf[:, j * 64:(j + 1) * 64], ones256[:], t % 2 == 0, t % 2 == 1,
                       r=[knf, ones256], w=[pkm[j]])
                if t % 2 == 1:
                    for j in range(2):
                        cp(C, "dve", kmT[:, j, t // 2:t // 2 + 1], pkm[j][:, 0:1], r=[pkm[j]], w=[kmT])
        if not nsa:
            C.dma("sp", d["KM"].rearrange("g d n -> d g n"), kmT[:], r=[kmT])
    C.P.barrier()


def attn_qk(C, ps_s, QTh, QTb, KTt, KTb, Em, negT, negTb, Emb, bias_ap, biasb, Pt, sel, ncols=512, col0=0):
    cs = slice(col0, col0 + ncols)
    has_m = Em is not None
    mm(C, ps_s[:, cs], KTt, QTh[:, cs], True, not has_m, r=[KTb, QTb], w=[ps_s])
    if has_m:
        mm(C, ps_s[:, cs], Em, negT[:, cs], False, True, r=[Emb, negTb], w=[ps_s])
    act(C, Pt[:, cs], ps_s[:, cs], AF.Exp, r=[ps_s, biasb], w=[Pt], bias=bias_ap)
    if sel is not None:
        pattern, base, cm = sel
        asel(C, Pt[:, cs], Pt[:, cs], pattern, base, cm, r=[Pt], w=[Pt])


def attn_pv(C, Pt, Vt, Vb, ps_o, osubs):
    nv = Vt.shape[-1]
    for (s, st_, sp_) in osubs:
        mm(C, ps_o[s][:, 0:nv], Pt[:, s * 128:(s + 1) * 128], Vt, st_, sp_, r=[Pt, Vb], w=[ps_o[s]])


class Pipe:
    def __init__(self, depth=2):
        self.depth = depth
        self.pend = []

    def push(self, qk, pv):
        qk()
        self.pend.append(pv)
        if len(self.pend) > self.depth:
            self.pend.pop(0)()

    def flush(self):
        for f in self.pend:
            f()
        self.pend = []


def attn_tile(C, pipe, ps_s, QTh, QTb, KTt, KTb, Em, negT, negTb, Emb, bias_ap, biasb, Pt, sel, Vt, Vb, ps_o, osubs,
              ncols=512, col0=0):
    pipe.push(lambda: attn_qk(C, ps_s, QTh, QTb, KTt, KTb, Em, negT, negTb, Emb, bias_ap, biasb, Pt, sel, ncols, col0),
              lambda: attn_pv(C, Pt, Vt, Vb, ps_o, osubs))


def attn_moba(C, d):
    with ExitStack() as st:
        identb = C.sb(st, [128, 128], BF16, "identb")
        C.dma("sp", identb[:], d["identb"], w=[identb])
        biask = C.sb(st, [128, 8, 36], F32, "biask")
        C.dma("sp", biask[:], d["biask"], w=[biask])
        addM = C.sb(st, [128, 32, 16], F32, "addM")
        C.dma("sp", addM[:], d["addM"].rearrange("t p n -> p t n"), w=[addM])
        own01 = C.sb(st, [128, 32, 16], F32, "own01")
        C.dma("sp", own01[:], d["own01"].rearrange("t p n -> p t n"), w=[own01])
        kmf = C.sb(st, [64, 2, 16], F32, "kmf")
        C.dma("sp", kmf[:], d["KM"].rearrange("g d n -> d g n"), w=[kmf])
        kmb = C.sb(st, [64, 2, 16], BF16, "kmb")
        cp(C, "dve", kmb[:], kmf[:], r=[kmf], w=[kmb])
        KT = C.sb(st, [80, 4096], BF16, "KT")
        C.dma("sp", KT[64:80, :], d["Emoba"].rearrange("n t m -> n (t m)"), w=[KT])
        V = C.sb(st, [128, 32, 65], BF16, "V")
        QT = [C.sb(st, [80, 4, 512], BF16, "QT%d" % i, nsub=4) for i in range(2)]
        qshB = C.sb(st, [80, 4, 512], BF16, "qshB")
        Pt = [C.sb(st, [128, 512], BF16, "Pt%d" % i) for i in range(4)]
        sgm = C.sb(st, [128, 4, 16], F32, "sgm")
        m8 = C.sb(st, [128, 4, 8], F32, "m8")
        thr = C.sb(st, [128, 4], F32, "thr")
        negm = [C.sb(st, [128, 4, 80], BF16, "negm%d" % i) for i in range(2)]
        for i in range(2):
            memset(C, "pool", negm[i][:], 0.0, w=[negm[i]])
        rden = C.sb(st, [128, 4], F32, "rden")
        acc = [C.sb(st, [128, 4, 256], BF16, "acc%d" % i) for i in range(2)]
        ps_s = [C.ps(st, [128, 512], F32, "ps_s%d" % i) for i in range(2)]
        ps_o = [C.ps(st, [128, 512], F32, "ps_o%d" % i) for i in range(4)]
        ps_g = C.ps(st, [128, 4, 16], F32, "ps_g")
        ps_t = C.ps(st, [80, 512], BF16, "ps_t")
        npt = 0
        nq = 0
        nh = 0
        pipe = Pipe(2)
        for g in range(2):
            C.dma("sp", KT[0:64, :], d["KT"][g, 0:64, :], w=[KT])
            C.dma("sp", V[:], d["V"][g].rearrange("(kt p) e -> p kt e", p=128), w=[V])
            for hh in range(4):
                C.dma("sp", qshB[64:80, hh, :], d["qshift"][4 * g + hh].partition_broadcast(16), w=[qshB])
            for qc in range(8):
                Q0 = qc * 512
                Q = QT[nq % 2]
                a_c = acc[nq % 2]
                nq += 1
                C.dma("sp", Q[0:64, :, :], d["QT"][4 * g:4 * g + 4, 0:64, Q0:Q0 + 512].rearrange("h d t -> d h t"),
                      w=Q.bs)
                for hh in range(4):
                    h = 4 * g + hh
                    n_m = negm[nh % 2]
                    nh += 1
                    Qb = Q.bs[hh]
                    for s in range(4):
                        mm(C, ps_g[:, s, :], Q[0:64, hh, s * 128:(s + 1) * 128], kmb[:, g, :], True, True,
                           r=[Qb, kmb], w=[ps_g])
                    tt(C, "dve", sgm[:], ps_g[:], addM[:, 4 * qc:4 * qc + 4, :], ALU.add, r=[ps_g, addM], w=[sgm])
                    for s in range(4):
                        C.op("dve", lambda e, s=s: e.max(out=m8[:, s, :], in_=sgm[:, s, :]), r=[sgm], w=[m8])
                    ts(C, "dve", thr[:], m8[:, :, 2], -1e29, None, ALU.max, None, r=[m8], w=[thr])
                    tt(C, "dve", sgm[:], sgm[:], thr[:].unsqueeze(2).to_broadcast([128, 4, 16]), ALU.is_ge,
                       r=[sgm, thr], w=[sgm])
                    tt(C, "dve", sgm[:], sgm[:], own01[:, 4 * qc:4 * qc + 4, :], ALU.max, r=[sgm, own01], w=[sgm])
                    ts(C, "dve", n_m[:, :, 64:80], sgm[:], -1.0, -NEG, ALU.add, ALU.mult, r=[sgm], w=[n_m])
                    for s in range(4):
                        tr(C, ps_t[:, s * 128:(s + 1) * 128], n_m[:, s, :], identb[:], r=[n_m, identb], w=[ps_t])
                    tt(C, "dve", Q[64:80, hh, :], ps_t[64:80, :], qshB[64:80, hh, :], ALU.add, r=[ps_t, qshB], w=[Qb])
                    for kt in range(4 * qc + 4):
                        P_ = Pt[npt % 4]
                        p_s = ps_s[npt % 2]
                        npt += 1
                        sel = ([[1, 512]], Q0 - 128 * kt, -1) if kt >= 4 * qc else None
                        osubs = [(s, kt == 0, kt == 4 * qc + s) for s in range(4) if kt <= 4 * qc + s]
                        attn_tile(C, pipe, p_s, Q[:, hh, :], Qb, KT[:, kt * 128:(kt + 1) * 128], KT, None, None,
                                  None, None, biask[:, h, kt - 4 * qc + 32:kt - 4 * qc + 33], biask, P_, sel,
                                  V[:, kt, :], V, ps_o, osubs)
                    pipe.flush()
                    for s in range(4):
                        C.op("dve", lambda e, s=s: e.reciprocal(out=rden[:, s:s + 1], in_=ps_o[s][:, 64:65]),
                             r=[ps_o[s]], w=[rden])
                        ts(C, "dve", a_c[:, s, hh * 64:(hh + 1) * 64], ps_o[s][:, 0:64], rden[:, s:s + 1], None,
                           ALU.mult, None, r=[ps_o[s], rden], w=[a_c])
                C.dma("sp", d["o"][Q0:Q0 + 512, g * 256:(g + 1) * 256].rearrange("(s p) c -> p s c", p=128),
                      a_c[:], r=[a_c])
    C.P.barrier()


def compress_stage(C, d, kcT, vcaug, identb):
    with ExitStack() as st:
        rawT = [C.sb(st, [64, 4096], BF16, "rawT%d" % i) for i in range(2)]
        wst = [C.sb(st, [64, 8, 256], F32, "cwst%d" % i) for i in range(2)]
        w2f = C.sb(st, [128, 2, 64], F32, "w2f")
        W1b = C.sb(st, [64, 32, 256], BF16, "W1b", nsub=4)
        W2b = C.sb(st, [128, 2, 64], BF16, "W2b")
        posf = C.sb(st, [64, 32], F32, "posf")
        posT = C.sb(st, [64, 32], BF16, "posT")
        c1s = C.sb(st, [128, 2], F32, "c1s")
        u = C.sb(st, [128, 256], F32, "u")
        u2 = C.sb(st, [128, 256], F32, "u2")
        sgt = C.sb(st, [128, 256], F32, "sgt")
        g1T = C.sb(st, [128, 2, 256], BF16, "g1T")
        gkc = C.sb(st, [128, 64], F32, "gkc")
        sq = C.sb(st, [128, 64], F32, "csq")
        ssq = C.sb(st, [128, 8], F32, "cssq")
        knc = C.sb(st, [128, 64], BF16, "knc")
        ovl = C.sb(st, [128, 2, 64], BF16, "ovl")
        ps_c1 = C.ps(st, [128, 2], F32, "ps_c1")
        ps_h = [C.ps(st, [128, 512], F32, "ps_h%d" % i) for i in range(2)]
        ps_k = C.ps(st, [128, 64], F32, "ps_k")
        ps_tk = C.ps(st, [64, 128], BF16, "ps_tk")
        C.dma("sp", gkc[:], d["k_norm"][0].partition_broadcast(128), w=[gkc])
        C.dma("sp", ovl[:], d["overlap"].rearrange("(t p) j -> p t j", p=128), w=[ovl])
        memset(C, "pool", g1T[:], 0.0, w=[g1T])
        nr = 0
        for kind in range(2):
            pre = "ck" if kind == 0 else "cv"
            w1v = d[pre + "_w1"].rearrange("(l dd) h -> dd l h", dd=64)
            for i in range(4):
                w_s = wst[i % 2]
                C.dma("sp", w_s[:], w1v[:, 8 * i:8 * i + 8, :], w=[w_s])
                cp(C, "pool", W1b[:, 8 * i:8 * i + 8, :], w_s[:], r=[w_s], w=[W1b.bs[i]])
            C.dma("sp", w2f[:], d[pre + "_w2"].rearrange("(c p) dd -> p c dd", p=128), w=[w2f])
            cp(C, "dve", W2b[:], w2f[:], r=[w2f], w=[W2b])
            C.dma("sp", posf[:], d[pre + "_pos"].rearrange("l dd -> dd l"), w=[posf], allow_slow_non_contiguous=True)
            cp(C, "dve", posT[:], posf[:], r=[posf], w=[posT])
            for hc in range(2):
                for l in range(32):
                    mm(C, ps_c1[:, hc:hc + 1], W1b[:, l, hc * 128:(hc + 1) * 128], posT[:, l:l + 1], l == 0, l == 31,
                       r=[W1b.bs[l // 8], posT], w=[ps_c1])
            cp(C, "dve", c1s[:], ps_c1[:], r=[ps_c1], w=[c1s])
            for g in range(2):
                r_T = rawT[nr % 2]
                nr += 1
                C.dma("sp", r_T[:], d["KT"][4 + 2 * kind + g, 0:64, :], w=[r_T])
                r3 = r_T[:].rearrange("p (n s) -> p n s", s=16)
                for hc in range(2):
                    for l in range(32):
                        rhs = r3[:, 0:255, l] if l < 16 else r3[:, 1:256, l - 16]
                        mm(C, ps_h[hc][:, 0:255], W1b[:, l, hc * 128:(hc + 1) * 128], rhs, l == 0, l == 31,
                           r=[W1b.bs[l // 8], r_T], w=[ps_h[hc]])
                    ts(C, "dve", u[:, 0:255], ps_h[hc][:, 0:255], c1s[:, hc:hc + 1], None, ALU.add, None,
                       r=[ps_h[hc], c1s], w=[u])
                    tt(C, "pool", u2[:, 0:255], u[:, 0:255], u[:, 0:255], ALU.mult, r=[u], w=[u2])
                    ts(C, "pool", u2[:, 0:255], u2[:, 0:255], 0.044715, 1.0, ALU.mult, ALU.add, r=[u2], w=[u2])
                    tt(C, "pool", u2[:, 0:255], u2[:, 0:255], u[:, 0:255], ALU.mult, r=[u2, u], w=[u2])
                    act(C, sgt[:, 0:255], u2[:, 0:255], AF.Sigmoid, r=[u2], w=[sgt], scale=1.5957691216057308)
                    tt(C, "dve", g1T[:, hc, 0:255], u[:, 0:255], sgt[:, 0:255], ALU.mult, r=[u, sgt], w=[g1T])
                for nt in range(2):
                    for hc in range(2):
                        mm(C, ps_k[:], g1T[:, hc, nt * 128:(nt + 1) * 128], W2b[:, hc, :], hc == 0, hc == 1,
                           r=[g1T, W2b], w=[ps_k])
                    if kind == 0:
                        headnorm(C, ps_k[:], 1, gkc, 1.0, sq, ssq, [(knc[:], knc)], ps_k)
                        tr(C, ps_tk[:], knc[:], identb[:], r=[knc, identb], w=[ps_tk])
                        act(C, kcT[g][0:64, nt * 128:(nt + 1) * 128], ps_tk[:], AF.Copy, r=[ps_tk], w=[kcT[g]])
                    else:
                        act(C, vcaug[g][:, nt, 0:64], ps_k[:], AF.Copy, r=[ps_k], w=[vcaug[g]])
        for g in range(2):
            memset(C, "pool", kcT[g][64:65, :], 1.0, w=[kcT[g]])
            memset(C, "pool", vcaug[g][:, :, 128:129], 1.0, w=[vcaug[g]])
            cp(C, "pool", vcaug[g][:, :, 64:128], ovl[:], r=[ovl], w=[vcaug[g]])
    C.P.barrier()


def attn_nsa(C, d):
    with ExitStack() as st0:
        identb = C.sb(st0, [128, 128], BF16, "identb")
        C.dma("sp", identb[:], d["identb"], w=[identb])
        kcT = [C.sb(st0, [65, 256], BF16, "kcT%d" % i) for i in range(2)]
        vcaug = [C.sb(st0, [128, 2, 129], BF16, "vcaug%d" % i) for i in range(2)]
        compress_stage(C, d, kcT, vcaug, identb)
        with ExitStack() as st:
            biask = C.sb(st, [128, 8, 36], F32, "biask")
            C.dma("sp", biask[:], d["biask"], w=[biask])
            biasc = C.sb(st, [128, 8, 8, 2], F32, "biasc")
            C.dma("sp", biasc[:], d["biasc"], w=[biasc])
            KsT = C.sb(st, [128, 4096], BF16, "KsT")
            C.dma("sp", KsT[64:128, :], d["Esel"].rearrange("n t m -> n (t m)"), w=[KsT])
            Qs = [C.sb(st, [128, 4, 512], BF16, "Qs%d" % i, nsub=4) for i in range(2)]
            qshB = C.sb(st, [128, 4, 512], BF16, "qshB")
            KwT = C.sb(st, [65, 4096], BF16, "KwT")
            Vs = C.sb(st, [128, 32, 65], BF16, "Vs")
            Vw = C.sb(st, [128, 32, 65], BF16, "Vw")
            QT = [C.sb(st, [65, 4, 512], BF16, "QT%d" % i) for i in range(2)]
            gt = [C.sb(st, [128, 4, 12], F32, "gt%d" % i) for i in range(2)]
            addT = [C.sb(st, [128, 4, 64], F32, "addT%d" % i) for i in range(2)]
            Pt = [C.sb(st, [128, 512], BF16, "Pt%d" % i) for i in range(4)]
            acc = C.sb(st, [128, 4, 256], F32, "acc")
            accb = [C.sb(st, [128, 4, 256], BF16, "accb%d" % i) for i in range(2)]
            imp = C.sb(st, [128, 4, 64], F32, "imp")
            sc = C.sb(st, [128, 64], F32, "sc")
            sc2 = C.sb(st, [128, 64], F32, "sc2")
            m8 = C.sb(st, [128, 16], F32, "m8")
            negm = C.sb(st, [128, 128], BF16, "negm")
            memset(C, "pool", negm[:], 0.0, w=[negm])
            sm = C.sb(st, [128, 16], F32, "sm")
            ps_s = [C.ps(st, [128, 512], F32, "ps_s%d" % i) for i in range(2)]
            ps_o = [C.ps(st, [128, 512], F32, "ps_o%d" % i) for i in range(4)]
            ps_t = C.ps(st, [128, 512], BF16, "ps_t")
            npt = 0
            nq = 0
            pipe = Pipe(2)
            for g in range(2):
                C.dma("sp", KsT[0:64, :], d["KT"][g, 0:64, :], w=[KsT])
                C.dma("sp", KwT[0:64, :], d["KT"][2 + g, 0:64, :], w=[KwT])
                for hh in range(4):
                    C.dma("sp", qshB[64:128, hh, :], d["qshift"][4 * g + hh].partition_broadcast(64), w=[qshB])
                memset(C, "pool", KwT[64:65, :], 1.0, w=[KwT])
                C.dma("sp", Vs[:], d["V"][g].rearrange("(kt p) e -> p kt e", p=128), w=[Vs])
                C.dma("sp", Vw[:], d["V"][2 + g].rearrange("(kt p) e -> p kt e", p=128), w=[Vw])
                for qc in range(8):
                    Q0 = qc * 512
                    Q = QT[nq % 2]
                    Q_s = Qs[nq % 2]
                    g_t = gt[nq % 2]
                    a_T = addT[nq % 2]
                    a_b = accb[nq % 2]
                    nq += 1
                    C.dma("sp", Q[0:64, :, :],
                          d["QT"][4 * g:4 * g + 4, 0:64, Q0:Q0 + 512].rearrange("h dd t -> dd h t"), w=[Q])
                    C.dma("sp", Q[64:65, :, :], d["qshift"][4 * g:4 * g + 4, :].unsqueeze(0), w=[Q])
                    C.dma("sp", Q_s[0:64, :, :],
                          d["QT"][4 * g:4 * g + 4, 0:64, Q0:Q0 + 512].rearrange("h dd t -> dd h t"), w=Q_s.bs)
                    C.dma("sp", g_t[:], d["G"][Q0:Q0 + 512, 12 * g:12 * g + 12].rearrange("(s p) c -> p s c", p=128),
                          w=[g_t])
                    C.dma("sp", a_T[:], d["addT"][4 * qc:4 * qc + 4].rearrange("t p n -> p t n"), w=[a_T])
                    nts = 2 if qc >= 4 else 1
                    for hh in range(4):
                        h = 4 * g + hh
                        for nt in range(nts):
                            P_ = Pt[npt % 4]
                            p_s = ps_s[npt % 2]
                            npt += 1
                            osubs = [(s, nt == 0, nt == nts - 1) for s in range(4)]
                            attn_tile(C, pipe, p_s, Q[:, hh, :], Q, kcT[g][:, nt * 128:(nt + 1) * 128], kcT[g], None, None,
                                      None, None, biasc[:, h, qc, nt:nt + 1], biasc, P_,
                                      ([[1, 512]], Q0 - 31 - 2048 * nt, -16), vcaug[g][:, nt, :], vcaug[g], ps_o,
                                      osubs)
                        pipe.flush()
                        for s in range(4):
                            ts(C, "dve", sm[:, 0:1], ps_o[s][:, 128:129], 1e-30, None, ALU.max, None,
                               r=[ps_o[s]], w=[sm])
                            C.op("dve", lambda e: e.reciprocal(out=sm[:, 1:2], in_=sm[:, 0:1]), r=[sm], w=[sm])
                            tt(C, "dve", sm[:, 2:3], sm[:, 1:2], g_t[:, s, 3 * hh:3 * hh + 1], ALU.mult,
                               r=[sm, g_t], w=[sm])
                            ts(C, "dve", acc[:, s, hh * 64:(hh + 1) * 64], ps_o[s][:, 0:64], sm[:, 2:3], None,
                               ALU.mult, None, r=[ps_o[s], sm], w=[acc])
                            if hh == 0:
                                ts(C, "dve", imp[:, s, :], ps_o[s][:, 64:128], sm[:, 1:2], None, ALU.mult, None,
                                   r=[ps_o[s], sm], w=[imp])
                            else:
                                stt(C, imp[:, s, :], ps_o[s][:, 64:128], sm[:, 1:2], imp[:, s, :], ALU.mult, ALU.add,
                                    r=[ps_o[s], sm, imp], w=[imp])
                    for s in range(4):
                        tt(C, "dve", sc[:], imp[:, s, :], a_T[:, s, :], ALU.add, r=[imp, a_T], w=[sc])
                        C.op("dve", lambda e: e.max(out=m8[:, 0:8], in_=sc[:]), r=[sc], w=[m8])
                        C.op("dve", lambda e: e.match_replace(out=sc2[:], in_to_replace=m8[:, 0:8], in_values=sc[:],
                                                              imm_value=-3.0e38), r=[sc, m8], w=[sc2])
                        C.op("dve", lambda e: e.max(out=m8[:, 8:16], in_=sc2[:]), r=[sc2], w=[m8])
                        ts(C, "dve", sm[:, 4:5], m8[:, 15:16], -1e29, None, ALU.max, None, r=[m8], w=[sm])
                        ts(C, "dve", sc2[:], sc[:], sm[:, 4:5], None, ALU.is_ge, None, r=[sc, sm], w=[sc2])
                        ts(C, "dve", negm[:, 64:128], sc2[:], -1.0, -NEG, ALU.add, ALU.mult, r=[sc2], w=[negm])
                        tr(C, ps_t[:, s * 128:(s + 1) * 128], negm[:], identb[:], r=[negm, identb], w=[ps_t])
                    for hh in range(4):
                        tt(C, "dve", Q_s[64:128, hh, :], ps_t[64:128, :], qshB[64:128, hh, :], ALU.add,
                           r=[ps_t, qshB], w=[Q_s.bs[hh]])
                    for hh in range(4):
                        h = 4 * g + hh
                        for kt in range(4 * qc + 4):
                            P_ = Pt[npt % 4]
                            p_s = ps_s[npt % 2]
                            npt += 1
                            sel = ([[1, 512]], Q0 - 128 * kt, -1) if kt >= 4 * qc else None
                            osubs = [(s, kt == 0, kt == 4 * qc + s) for s in range(4) if kt <= 4 * qc + s]
                            attn_tile(C, pipe, p_s, Q_s[:, hh, :], Q_s.bs[hh], KsT[:, kt * 128:(kt + 1) * 128], KsT,
                                      None, None, None, None, biask[:, h, kt - 4 * qc + 32:kt - 4 * qc + 33], biask,
                                      P_, sel, Vs[:, kt, :], Vs, ps_o, osubs)
                        pipe.flush()
                        for s in range(4):
                            C.op("dve", lambda e, s=s: e.reciprocal(out=sm[:, 1:2], in_=ps_o[s][:, 64:65]),
                                 r=[ps_o[s]], w=[sm])
                            tt(C, "dve", sm[:, 2:3], sm[:, 1:2], g_t[:, s, 3 * hh + 1:3 * hh + 2], ALU.mult,
                               r=[sm, g_t], w=[sm])
                            stt(C, acc[:, s, hh * 64:(hh + 1) * 64], ps_o[s][:, 0:64], sm[:, 2:3],
                                acc[:, s, hh * 64:(hh + 1) * 64], ALU.mult, ALU.add, r=[ps_o[s], sm, acc], w=[acc])
                    for hh in range(4):
                        h = 4 * g + hh
                        for kt in range(max(0, 4 * qc - 4), 4 * qc + 4):
                            rel = kt - 4 * qc
                            s_lo, s_hi = max(0, rel), min(3, rel + 4)
                            col0, ncols = 128 * s_lo, 128 * (s_hi - s_lo + 1)
                            P_ = Pt[npt % 4]
                            p_s = ps_s[npt % 2]
                            npt += 1
                            if rel < 0:
                                sel = ([[-1, ncols]], 128 * kt - Q0 + 511 - col0, 1)
                            else:
                                sel = ([[1, ncols]], Q0 - 128 * kt + col0, -1)
                            osubs = [(s, kt == max(0, 4 * qc + s - 4), kt == 4 * qc + s) for s in range(s_lo, s_hi + 1)]
                            attn_tile(C, pipe, p_s, Q[:, hh, :], Q, KwT[:, kt * 128:(kt + 1) * 128], KwT, None, None, None,
                                      None, biask[:, h, rel + 32:rel + 33], biask, P_, sel, Vw[:, kt, :], Vw, ps_o,
                                      osubs, ncols=ncols, col0=col0)
                        pipe.flush()
                        for s in range(4):
                            C.op("dve", lambda e, s=s: e.reciprocal(out=sm[:, 1:2], in_=ps_o[s][:, 64:65]),
                                 r=[ps_o[s]], w=[sm])
                            tt(C, "dve", sm[:, 2:3], sm[:, 1:2], g_t[:, s, 3 * hh + 2:3 * hh + 3], ALU.mult,
                               r=[sm, g_t], w=[sm])
                            stt(C, acc[:, s, hh * 64:(hh + 1) * 64], ps_o[s][:, 0:64], sm[:, 2:3],
                                acc[:, s, hh * 64:(hh + 1) * 64], ALU.mult, ALU.add, r=[ps_o[s], sm, acc], w=[acc])
                    act(C, a_b[:], acc[:], AF.Copy, r=[acc], w=[a_b])
                    C.dma("sp", d["o"][Q0:Q0 + 512, g * 256:(g + 1) * 256].rearrange("(s p) c -> p s c", p=128),
                          a_b[:], r=[a_b])
    C.P.barrier()


import ml_dtypes
from concourse.bass_utils import run_bass_kernel_spmd

_bf = ml_dtypes.bfloat16


def _consts_common(hp):
    slopes = 2.0 ** (-(np.arange(16) + 1) / 2.0)
    sl = slopes[8 * hp:8 * hp + 8]
    qshift = (-sl[:, None] * np.arange(512)[None, :]).astype(np.float32).astype(_bf)
    j = np.arange(36) - 32
    biask = (sl[None, :, None] * (128.0 * j[None, None, :] + np.arange(128)[:, None, None])).astype(np.float32)
    return dict(qshift=qshift, biask=biask, identb=np.eye(128, dtype=np.float32).astype(_bf),
                identf=np.eye(128, dtype=np.float32))


def _consts_moba():
    E = np.zeros((16, 32, 128), np.float32)
    for kt in range(32):
        E[kt // 2, kt, :] = 1
    addM = np.zeros((32, 128, 16), np.float32)
    own = np.zeros((32, 128, 16), np.float32)
    for qt in range(32):
        cb = qt // 2
        addM[qt, :, cb:] = -1e30
        own[qt, :, cb] = 1
    return dict(Emoba=E.astype(_bf), addM=addM, own01=own)


def _consts_nsa(hp):
    slopes = 2.0 ** (-(np.arange(16) + 1) / 2.0)
    sl = slopes[8 * hp:8 * hp + 8]
    E = np.zeros((64, 32, 128), np.float32)
    for kt in range(32):
        E[2 * kt, kt, 0:64] = 1
        E[2 * kt + 1, kt, 64:128] = 1
    p = np.arange(128)
    biasc = np.zeros((128, 8, 8, 2), np.float32)
    for qc in range(8):
        for nt in range(2):
            biasc[:, :, qc, nt] = sl[None, :] * (16.0 * (p[:, None] + 128 * nt) + 31 - 512 * qc)
    addT = np.zeros((32, 128, 64), np.float32)
    for qt in range(32):
        for half in range(2):
            cur = 2 * qt + half
            rows = slice(64 * half, 64 * half + 64)
            addT[qt, rows, cur + 1:] = -1e30
            for j in (0, cur, cur - 1):
                if j >= 0:
                    addT[qt, rows, j] = 1e9
    n = np.arange(256)[:, None]
    j = np.arange(64)[None, :]
    ov = np.clip(np.minimum(16 * n + 32, 64 * j + 64) - np.maximum(16 * n, 64 * j), 0, None) / 32.0
    ov[255] = 0
    return dict(Esel=E.astype(_bf), biasc=biasc, addT=addT, overlap=ov.astype(np.float32).astype(_bf))


def _din(nc, name, shape, dt=F32):
    return nc.dram_tensor(name, list(shape), dt, kind="ExternalInput").ap()


def _dint(nc, name, shape, dt):
    return nc.dram_tensor(name, list(shape), dt, kind="Internal").ap()


def _decl_A0(nc):
    return dict(x=_din(nc, "x", [4096, 1024]), wcat=_din(nc, "wcat", [1024, 1304]), ln_mix=_din(nc, "ln_mix", [1024]),
                q_norm=_din(nc, "q_norm", [64]), k_norm=_din(nc, "k_norm", [3, 64]),
                ck_pos=_din(nc, "ck_pos", [32, 64]), ck_w1=_din(nc, "ck_w1", [2048, 256]),
                ck_w2=_din(nc, "ck_w2", [256, 64]), cv_pos=_din(nc, "cv_pos", [32, 64]),
                cv_w1=_din(nc, "cv_w1", [2048, 256]), cv_w2=_din(nc, "cv_w2", [256, 64]),
                identb=_din(nc, "identb", [128, 128], BF16), identf=_din(nc, "identf", [128, 128]),
                qshift=_din(nc, "qshift", [8, 512], BF16), biask=_din(nc, "biask", [128, 8, 36]),
                Esel=_din(nc, "Esel", [64, 32, 128], BF16), biasc=_din(nc, "biasc", [128, 8, 8, 2]),
                addT=_din(nc, "addT", [32, 128, 64]), overlap=_din(nc, "overlap", [256, 64], BF16),
                QT=_dint(nc, "QT", [8, 64, 4096], BF16), KT=_dint(nc, "KT", [8, 64, 4096], BF16),
                V=_dint(nc, "V", [4, 4096, 65], BF16), G=_dint(nc, "G", [4096, 24], F32))


def _decl_A1(nc):
    return dict(x=_din(nc, "x", [4096, 1024]), wq=_din(nc, "wq", [1024, 512]), wkv=_din(nc, "wkv", [1024, 256]),
                ln_mix=_din(nc, "ln_mix", [1024]), kv_norm=_din(nc, "kv_norm", [1024]),
                q_norm=_din(nc, "q_norm", [64]), k_norm=_din(nc, "k_norm", [64]),
                identb=_din(nc, "identb", [128, 128], BF16), identf=_din(nc, "identf", [128, 128]),
                qshift=_din(nc, "qshift", [8, 512], BF16), biask=_din(nc, "biask", [128, 8, 36]),
                Emoba=_din(nc, "Emoba", [16, 32, 128], BF16), addM=_din(nc, "addM", [32, 128, 16]),
                own01=_din(nc, "own01", [32, 128, 16]),
                QT=_dint(nc, "QT", [8, 64, 4096], BF16), KT=_dint(nc, "KT", [2, 64, 4096], BF16),
                V=_dint(nc, "V", [2, 4096, 65], BF16), KM=_dint(nc, "KM", [2, 64, 16], F32))


def _decl_B(nc, NT):
    return dict(hin=_din(nc, "hin", [NT, 1024]), o=_din(nc, "o", [NT, 1024], BF16), p=_din(nc, "p", [NT, 256]),
                w_out=_din(nc, "w_out", [1024, 1024]), ln_ffn=_din(nc, "ln_ffn", [1024]),
                ln_ple=_din(nc, "ln_ple", [1024]), w_group=_din(nc, "w_group", [1024, 4]),
                b_group=_din(nc, "b_group", [4]), w_expert=_din(nc, "w_expert", [1024, 32]),
                b_expert=_din(nc, "b_expert", [32]), w_gate=_din(nc, "w_gate", [32, 1024, 128]),
                w_up=_din(nc, "w_up", [32, 1024, 128]), w_down=_din(nc, "w_down", [32, 128, 1024]),
                ple_w_proj=_din(nc, "ple_w_proj", [256, 1024]), ple_w_gate=_din(nc, "ple_w_gate", [1024, 1024]),
                identb=_din(nc, "identb", [128, 128], BF16), identf=_din(nc, "identf", [128, 128]),
                sele=_din(nc, "sele", [32, 32, 128], BF16))


def _build(which):
    nc = bass.Bass("TRN2", target_bir_lowering=False)
    with ExitStack() as st:
        P = Prog(nc, st)
        C = Ctx(nc, P)
        if which == "A0":
            d = _decl_A0(nc)
            d["o"] = nc.dram_tensor("o", [4096, 512], BF16, kind="ExternalOutput").ap()
            prep_v1(C, d, "nsa")
            attn_nsa(C, d)
        elif which == "A1":
            d = _decl_A1(nc)
            d["o"] = nc.dram_tensor("o", [4096, 512], BF16, kind="ExternalOutput").ap()
            prep(C, d, "moba")
            attn_moba(C, d)
        else:
            d = _decl_B(nc, 2048)
            d["hout"] = nc.dram_tensor("hout", [2048, 1024], F32, kind="ExternalOutput").ap()
            phase_B(C, 2048, d)
        P.emit()
    return nc


def _wcat_for(w, hp):
    q = w[:, 512 * hp:512 * hp + 512]

    def kv(i):
        base = 1024 + 256 * i
        return w[:, base + 128 * hp: base + 128 * hp + 128]
    kc, vc, ks, vs, kw, vw = [kv(i) for i in range(6)]
    gl = w[:, 2560 + 24 * hp: 2560 + 24 * hp + 24]
    return np.ascontiguousarray(np.concatenate([q, ks, kw, kc, vc, vs, vw, gl], 1))


def _run_B(I, L, h, o, w_out):
    sele = np.zeros((32, 32, 128), np.float32)
    for e in range(32):
        sele[e, e, :] = 1
    consts = dict(identb=np.eye(128, dtype=np.float32).astype(_bf), identf=np.eye(128, dtype=np.float32),
                  sele=sele.astype(_bf))
    maps = []
    for c in range(8):
        b, hf = c // 2, c % 2
        tk = slice(hf * 2048, hf * 2048 + 2048)
        maps.append(dict(hin=np.ascontiguousarray(h[b, tk]), o=np.ascontiguousarray(o[b, tk]),
                         p=np.ascontiguousarray(I['p'][L, b, tk]), w_out=w_out, ln_ffn=I['ln_ffn'][L],
                         ln_ple=I['ln_ple'][L], w_group=I['moe_w_group'][L], b_group=I['moe_b_group'][L],
                         w_expert=I['moe_w_expert'][L], b_expert=I['moe_b_expert'][L], w_gate=I['moe_w_gate'][L],
                         w_up=I['moe_w_up'][L], w_down=I['moe_w_down'][L], ple_w_proj=I['ple_w_proj'][L],
                         ple_w_gate=I['ple_w_gate'][L], **consts))
    res = run_bass_kernel_spmd(_build("B"), maps, core_ids=list(range(8)))
    out = np.empty((4, 4096, 1024), np.float32)
    for c in range(8):
        b, hf = c // 2, c % 2
        out[b, hf * 2048:hf * 2048 + 2048] = res.results[c]["hout"]
    return out


def _gather_o(res):
    o = np.empty((4, 4096, 1024), _bf)
    for c in range(8):
        b, hp = c // 2, c % 2
        o[b, :, 512 * hp:512 * hp + 512] = res.results[c]["o"]
    return o


def kernel(**inputs):
    I = {k: np.asarray(v) for k, v in inputs.items()}
    x = I['x']
    maps = []
    for c in range(8):
        b, hp = c // 2, c % 2
        maps.append(dict(x=np.ascontiguousarray(x[b]), wcat=_wcat_for(I['a_w_in'][0], hp), ln_mix=I['ln_mix'][0],
                         q_norm=I['a_q_norm'][0], k_norm=I['a_k_norm'][0], ck_pos=I['a_ck_pos'][0],
                         ck_w1=I['a_ck_w1'][0], ck_w2=I['a_ck_w2'][0], cv_pos=I['a_cv_pos'][0],
                         cv_w1=I['a_cv_w1'][0], cv_w2=I['a_cv_w2'][0], **_consts_common(hp), **_consts_nsa(hp)))
    o0 = _gather_o(run_bass_kernel_spmd(_build("A0"), maps, core_ids=list(range(8))))
    h0 = _run_B(I, 0, x, o0, I['a_w_out'][0])
    maps = []
    cm = _consts_moba()
    wkv = I['w_kv_shared']
    for c in range(8):
        b, hp = c // 2, c % 2
        maps.append(dict(x=np.ascontiguousarray(h0[b]),
                         wq=np.ascontiguousarray(I['b_w_q'][0][:, 512 * hp:512 * hp + 512]),
                         wkv=np.ascontiguousarray(np.concatenate(
                             [wkv[:, 128 * hp:128 * hp + 128], wkv[:, 256 + 128 * hp:256 + 128 * hp + 128]], 1)),
                         ln_mix=I['ln_mix'][1], kv_norm=I['kv_norm'], q_norm=I['b_q_norm'][0],
                         k_norm=I['k_norm_shared'], **_consts_common(hp), **cm))
    o1 = _gather_o(run_bass_kernel_spmd(_build("A1"), maps, core_ids=list(range(8))))
    return _run_B(I, 1, h0, o1, I['b_w_out'][0])


def _build_fused():
    nc = bass.Bass("TRN2", target_bir_lowering=False)
    d0 = dict(x=_din(nc, "x", [4096, 1024]), q_norm=_din(nc, "q_norm", [64]), k_norm=_din(nc, "k_norm", [3, 64]),
              ck_pos=_din(nc, "ck_pos", [32, 64]), ck_w1=_din(nc, "ck_w1", [2048, 256]),
              ck_w2=_din(nc, "ck_w2", [256, 64]), cv_pos=_din(nc, "cv_pos", [32, 64]),
              cv_w1=_din(nc, "cv_w1", [2048, 256]), cv_w2=_din(nc, "cv_w2", [256, 64]),
              identb=_din(nc, "identb", [128, 128], BF16), identf=_din(nc, "identf", [128, 128]),
              QT=_dint(nc, "QT", [8, 64, 4096], BF16), KT=_dint(nc, "KT", [8, 64, 4096], BF16),
              V=_dint(nc, "V", [4, 4096, 65], BF16), G=_dint(nc, "G", [4096, 24], F32))
    per_hp = []
    for hp in range(2):
        per_hp.append(dict(
            wcat=_din(nc, "wcat%d" % hp, [1024, 1304]), qshift=_din(nc, "qshift%d" % hp, [8, 512], BF16),
            biask=_din(nc, "biask%d" % hp, [128, 8, 36]), biasc=_din(nc, "biasc%d" % hp, [128, 8, 8, 2]),
            wq=_din(nc, "wq%d" % hp, [1024, 512]), wkv=_din(nc, "wkv%d" % hp, [1024, 256])))
    shared = dict(Esel=_din(nc, "Esel", [64, 32, 128], BF16), addT=_din(nc, "addT", [32, 128, 64]),
                  overlap=_din(nc, "overlap", [256, 64], BF16), Emoba=_din(nc, "Emoba", [16, 32, 128], BF16),
                  addM=_din(nc, "addM", [32, 128, 16]), own01=_din(nc, "own01", [32, 128, 16]),
                  sele=_din(nc, "sele", [32, 32, 128], BF16))
    ln_mix = _din(nc, "ln_mix_all", [2, 1024])
    ln_ffn = _din(nc, "ln_ffn_all", [2, 1024])
    ln_ple = _din(nc, "ln_ple_all", [2, 1024])
    pin = _din(nc, "p", [2, 4096, 256])
    kv_norm = _din(nc, "kv_norm", [1024])
    bq_norm = _din(nc, "b_q_norm", [64])
    ks_norm = _din(nc, "k_norm_shared", [64])
    w_out = [_din(nc, "w_out%d" % L, [1024, 1024]) for L in range(2)]
    moe = dict(w_group=_din(nc, "moe_w_group", [2, 1024, 4]), b_group=_din(nc, "moe_b_group", [2, 4]),
               w_expert=_din(nc, "moe_w_expert", [2, 1024, 32]), b_expert=_din(nc, "moe_b_expert", [2, 32]),
               w_gate=_din(nc, "moe_w_gate", [2, 32, 1024, 128]), w_up=_din(nc, "moe_w_up", [2, 32, 1024, 128]),
               w_down=_din(nc, "moe_w_down", [2, 32, 128, 1024]), ple_w_proj=_din(nc, "ple_w_proj", [2, 256, 1024]),
               ple_w_gate=_din(nc, "ple_w_gate", [2, 1024, 1024]))
    o_s = [_dint(nc, "o_s%d" % L, [4096, 1024], BF16) for L in range(2)]
    h0_s = _dint(nc, "h0_s", [4096, 1024], F32)
    KT1 = _dint(nc, "KT1", [2, 64, 4096], BF16)
    V1 = _dint(nc, "V1", [2, 4096, 65], BF16)
    KM1 = _dint(nc, "KM1", [2, 64, 16], F32)
    hout = nc.dram_tensor("hout", [4096, 1024], F32, kind="ExternalOutput").ap()
    x = d0["x"]
    with ExitStack() as st:
        P = Prog(nc, st)
        C = Ctx(nc, P)

        def run_B(L, hin, hdst):
            for half in range(2):
                tk = slice(half * 2048, half * 2048 + 2048)
                dB = dict(hin=hin[tk, :], o=o_s[L][tk, :], p=pin[L, tk, :], w_out=w_out[L], ln_ffn=ln_ffn[L],
                          ln_ple=ln_ple[L], w_group=moe["w_group"][L], b_group=moe["b_group"][L],
                          w_expert=moe["w_expert"][L], b_expert=moe["b_expert"][L], w_gate=moe["w_gate"][L],
                          w_up=moe["w_up"][L], w_down=moe["w_down"][L], ple_w_proj=moe["ple_w_proj"][L],
                          ple_w_gate=moe["ple_w_gate"][L], identb=d0["identb"], identf=d0["identf"],
                          sele=shared["sele"], hout=hdst[tk, :])
                phase_B(C, 2048, dB)

        for hp in range(2):
            dA = dict(d0)
            dA.update(shared)
            dA.update(per_hp[hp])
            dA["ln_mix"] = ln_mix[0]
            dA["o"] = o_s[0][:, 512 * hp:512 * hp + 512]
            prep_v1(C, dA, "nsa")
            attn_nsa(C, dA)
        run_B(0, x, h0_s)
        for hp in range(2):
            dA = dict(x=h0_s, ln_mix=ln_mix[1], kv_norm=kv_norm, q_norm=bq_norm, k_norm=ks_norm,
                      identb=d0["identb"], identf=d0["identf"], QT=d0["QT"], KT=KT1, V=V1, KM=KM1)
            dA.update(shared)
            dA.update(per_hp[hp])
            dA["o"] = o_s[1][:, 512 * hp:512 * hp + 512]
            prep(C, dA, "moba")
            attn_moba(C, dA)
        run_B(1, h0_s, hout)
        print("fused program instruction counts:", {e: len(q) for e, q in P.q.items()})
        P.emit()
    return nc


def kernel_fused(**inputs):
    I = {k: np.ascontiguousarray(np.asarray(v)) for k, v in inputs.items()}
    cm = _consts_moba()
    sele = np.zeros((32, 32, 128), np.float32)
    for e in range(32):
        sele[e, e, :] = 1
    wkv = I['w_kv_shared']
    base = dict(ln_mix_all=I['ln_mix'], ln_ffn_all=I['ln_ffn'], ln_ple_all=I['ln_ple'], kv_norm=I['kv_norm'],
                b_q_norm=I['b_q_norm'][0], k_norm_shared=I['k_norm_shared'], w_out0=I['a_w_out'][0],
                w_out1=I['b_w_out'][0], moe_w_group=I['moe_w_group'], moe_b_group=I['moe_b_group'],
                moe_w_expert=I['moe_w_expert'], moe_b_expert=I['moe_b_expert'], moe_w_gate=I['moe_w_gate'],
                moe_w_up=I['moe_w_up'], moe_w_down=I['moe_w_down'], ple_w_proj=I['ple_w_proj'],
                ple_w_gate=I['ple_w_gate'], q_norm=I['a_q_norm'][0], k_norm=I['a_k_norm'][0],
                ck_pos=I['a_ck_pos'][0], ck_w1=I['a_ck_w1'][0], ck_w2=I['a_ck_w2'][0], cv_pos=I['a_cv_pos'][0],
                cv_w1=I['a_cv_w1'][0], cv_w2=I['a_cv_w2'][0], sele=sele.astype(_bf), **cm)
    cc = _consts_common(0)
    base["identb"], base["identf"] = cc["identb"], cc["identf"]
    for hp in range(2):
        cc = _consts_common(hp)
        cn = _consts_nsa(hp)
        base["wcat%d" % hp] = _wcat_for(I['a_w_in'][0], hp)
        base["qshift%d" % hp] = cc["qshift"]
        base["biask%d" % hp] = cc["biask"]
        base["biasc%d" % hp] = cn["biasc"]
        base["wq%d" % hp] = np.ascontiguousarray(I['b_w_q'][0][:, 512 * hp:512 * hp + 512])
        base["wkv%d" % hp] = np.ascontiguousarray(np.concatenate(
            [wkv[:, 128 * hp:128 * hp + 128], wkv[:, 256 + 128 * hp:256 + 128 * hp + 128]], 1))
        if hp == 0:
            base["Esel"], base["addT"], base["overlap"] = cn["Esel"], cn["addT"], cn["overlap"]
    maps = []
    for c in range(8):
        b = c // 2
        m = dict(base)
        m["x"] = I['x'][b]
        m["p"] = np.ascontiguousarray(I['p'][:, b])
        maps.append(m)
    res = run_bass_kernel_spmd(_build_fused(), maps, core_ids=list(range(8)))
    out = np.empty((4, 4096, 1024), np.float32)
    for c in range(8):
        b, hf = c // 2, c % 2
        out[b, hf * 2048:hf * 2048 + 2048] = res.results[c]["hout"][hf * 2048:hf * 2048 + 2048]
    return out
```
